# Optimizing a Trainium2 kernel written in Bass

```python
import math
import jax, jax.numpy as jnp
from jax import lax
import numpy as np

D_MODEL = 1024
BATCH = 4
SEQ = 4096
DEPTH = 1

CHUNK = 64
MIX_WIDTH = D_MODEL
POOL_WIDTH = MIX_WIDTH // 2
POOL_WINDOWS = (2, 4, 8, 16)
POOL_GROUP = POOL_WIDTH // len(POOL_WINDOWS)
ATTN_HEADS = 8
HEAD_DIM = (MIX_WIDTH - POOL_WIDTH) // ATTN_HEADS
ATTN_WIDTH = ATTN_HEADS * HEAD_DIM
IDX_HEADS = 8
IDX_DIM = 64
TOPK_MAX = 256
Q_BLOCK = 128
ROPE_THETA = 10000.0
MEM_TOKENS = 256
MEM_HEADS = 4
MEM_HEAD_DIM = 128
MEM_WIDTH = MEM_HEADS * MEM_HEAD_DIM
N_GROUPS = 4
EXPERTS_PER_GROUP = 8
EXPERT_FF = 256
TOP_K_EXPERTS = 2
EPS = 1e-6
OFF_POOL = 0
OFF_Q = OFF_POOL + POOL_WIDTH
OFF_K = OFF_Q + ATTN_WIDTH
OFF_V = OFF_K + ATTN_WIDTH
OFF_QI = OFF_V + ATTN_WIDTH
OFF_KI = OFF_QI + IDX_HEADS * IDX_DIM
OFF_WI = OFF_KI + IDX_DIM
IN_COLS = OFF_WI + IDX_HEADS

kernel_name = "hybrid_pool_dsa_hmoe_block"


def rmsnorm(x, g):
    xf = x.astype(jnp.float32)
    y = xf * lax.rsqrt(jnp.mean(xf * xf, axis=-1, keepdims=True) + EPS)
    return (y * g.astype(jnp.float32)).astype(x.dtype)


def rope_tables(seq_len, dim):
    inv = ROPE_THETA ** (-jnp.arange(0, dim, 2, dtype=jnp.float32) / dim)
    ang = jnp.arange(seq_len, dtype=jnp.float32)[:, None] * inv[None, :]
    ang = jnp.concatenate([ang, ang], axis=-1)
    return jnp.cos(ang), jnp.sin(ang)


def apply_rope(x, cos, sin):
    xf = x.astype(jnp.float32)
    half = xf.shape[-1] // 2
    rot = jnp.concatenate([-xf[..., half:], xf[..., :half]], axis=-1)
    return (xf * cos[None, :, None, :] + rot * sin[None, :, None, :]).astype(x.dtype)


def pool_mixer(u, pool_w, pool_scale):
    B, S, _ = u.shape
    uf = u.astype(jnp.float32)
    pos1 = jnp.arange(1, S + 1)
    outs = []
    for g, w in enumerate(POOL_WINDOWS):
        ug = uf[..., g * POOL_GROUP:(g + 1) * POOL_GROUP]
        cs = jnp.cumsum(ug, axis=1)
        lag = jnp.pad(cs, ((0, 0), (w, 0), (0, 0)))[:, :S]
        cnt = jnp.minimum(pos1, w).astype(jnp.float32)[None, :, None]
        mixed = ((cs - lag) / cnt - ug).astype(u.dtype)
        outs.append(jnp.einsum('bsc,cd->bsd', mixed, pool_w[g]))
    return jnp.concatenate(outs, axis=-1) * pool_scale


def dsa_attention(q, k, v, qi, ki, wi):
    B, S, H, Dh = q.shape
    topk = min(TOPK_MAX, S // 4)
    n_blocks = S // Q_BLOCK
    key_chunk = jnp.arange(S) // CHUNK
    idx_scale = (IDX_DIM ** -0.5) * (IDX_HEADS ** -0.5)
    gather = jax.vmap(lambda a, ix: a[ix])

    def block(i):
        start = i * Q_BLOCK
        qb = lax.dynamic_slice_in_dim(q, start, Q_BLOCK, axis=1)
        qib = lax.dynamic_slice_in_dim(qi, start, Q_BLOCK, axis=1)
        wib = lax.dynamic_slice_in_dim(wi, start, Q_BLOCK, axis=1)
        q_chunk = (start + jnp.arange(Q_BLOCK)) // CHUNK
        adm = key_chunk[None, :] <= q_chunk[:, None]
        rel = jax.nn.relu(jnp.einsum('bthd,bsd->bths', qib, ki).astype(jnp.float32))
        score = jnp.einsum('bths,bth->bts', rel, wib.astype(jnp.float32)) * idx_scale
        score = jnp.where(adm[None], score, -jnp.inf)
        _, sel = lax.top_k(score, topk)
        valid = key_chunk[sel] <= q_chunk[None, :, None]
        ks = gather(k, sel)
        vs = gather(v, sel)
        s = jnp.einsum('bthd,btkhd->bthk', qb, ks).astype(jnp.float32) * (Dh ** -0.5)
        s = jnp.where(valid[:, :, None, :], s, -jnp.inf)
        p = jax.nn.softmax(s, axis=-1).astype(v.dtype)
        return jnp.einsum('bthk,btkhd->bthd', p, vs)

    out = lax.map(block, jnp.arange(n_blocks))
    return out.transpose(1, 0, 2, 3, 4).reshape(B, S, H * Dh)


def memory_cross_attention(h, mem, g_mem, w_q, w_kv, q_norm, k_norm, w_o):
    B, S, _ = h.shape
    M = mem.shape[1]
    m = rmsnorm(mem, g_mem)
    q = (h @ w_q).reshape(B, S, MEM_HEADS, MEM_HEAD_DIM)
    kv = (m @ w_kv).reshape(B, M, 2, MEM_HEADS, MEM_HEAD_DIM)
    q = rmsnorm(q, q_norm)
    k = rmsnorm(kv[:, :, 0], k_norm)
    v = kv[:, :, 1]
    s = jnp.einsum('bshd,bmhd->bhsm', q, k).astype(jnp.float32) * (MEM_HEAD_DIM ** -0.5)
    p = jax.nn.softmax(s, axis=-1).astype(v.dtype)
    o = jnp.einsum('bhsm,bmhd->bshd', p, v).reshape(B, S, MEM_WIDTH)
    return o @ w_o


def hierarchical_moe(h, wg_router, bg_router, we_router, be_router, w_gate, w_up, w_down):
    B, S, D = h.shape
    hf = h.reshape(B * S, D)
    g_logits = (hf @ wg_router + bg_router).astype(jnp.float32)
    g_prob = jax.nn.softmax(g_logits, axis=-1)
    g_sel = jnp.argmax(g_logits, axis=-1)
    g_w = jnp.take_along_axis(g_prob, g_sel[:, None], axis=1)
    e_logits = (hf @ we_router + be_router).astype(jnp.float32)
    e_logits = e_logits.reshape(-1, N_GROUPS, EXPERTS_PER_GROUP)
    e_in = jnp.take_along_axis(e_logits, g_sel[:, None, None], axis=1)[:, 0]
    top_v, top_i = lax.top_k(e_in, TOP_K_EXPERTS)
    top_w = jax.nn.softmax(top_v, axis=-1) * g_w
    exp_w = jnp.sum(jax.nn.one_hot(top_i, EXPERTS_PER_GROUP, dtype=jnp.float32)
                    * top_w[..., None], axis=1)
    gates = jax.nn.one_hot(g_sel, N_GROUPS, dtype=jnp.float32)[:, :, None] * exp_w[:, None, :]
    out = jnp.zeros_like(hf)
    for g in range(N_GROUPS):
        a = jnp.einsum('nd,edf->nef', hf, w_gate[g])
        b = jnp.einsum('nd,edf->nef', hf, w_up[g])
        hid = jax.nn.silu(a) * b * gates[:, g, :, None].astype(hf.dtype)
        out = out + jnp.einsum('nef,efd->nd', hid, w_down[g])
    return out.reshape(B, S, D)


def setup_inputs(seed: int = 0) -> dict:
    key = jax.random.key(seed)
    ks = jax.random.split(key, 32)
    f32 = jnp.float32
    L = DEPTH

    def nrm(k, shape, scale):
        return jax.random.normal(k, shape, f32) * scale

    def gain(k, shape):
        return 1.0 + 0.02 * jax.random.normal(k, shape, f32)

    return {
        "x": nrm(ks[0], (BATCH, SEQ, D_MODEL), 1.0),
        "mem": nrm(ks[1], (BATCH, MEM_TOKENS, D_MODEL), 1.0),
        "mix_norm": gain(ks[2], (L, D_MODEL)),
        "w_in": nrm(ks[3], (L, D_MODEL, IN_COLS), D_MODEL ** -0.5),
        "pool_w": nrm(ks[4], (L, len(POOL_WINDOWS), POOL_GROUP, POOL_GROUP), POOL_GROUP ** -0.5),
        "pool_scale": gain(ks[5], (L, POOL_WIDTH)),
        "q_norm": gain(ks[6], (L, HEAD_DIM)),
        "k_norm": gain(ks[7], (L, HEAD_DIM)),
        "idx_k_norm": gain(ks[8], (L, IDX_DIM)),
        "w_out": nrm(ks[9], (L, MIX_WIDTH, D_MODEL), MIX_WIDTH ** -0.5),
        "xattn_norm": gain(ks[10], (L, D_MODEL)),
        "mem_norm": gain(ks[11], (L, D_MODEL)),
        "xattn_wq": nrm(ks[12], (L, D_MODEL, MEM_WIDTH), D_MODEL ** -0.5),
        "xattn_wkv": nrm(ks[13], (L, D_MODEL, 2 * MEM_WIDTH), D_MODEL ** -0.5),
        "xattn_q_norm": gain(ks[14], (L, MEM_HEAD_DIM)),
        "xattn_k_norm": gain(ks[15], (L, MEM_HEAD_DIM)),
        "xattn_wo": nrm(ks[16], (L, MEM_WIDTH, D_MODEL), MEM_WIDTH ** -0.5),
        "ffn_norm": gain(ks[17], (L, D_MODEL)),
        "router_group_w": nrm(ks[18], (L, D_MODEL, N_GROUPS), D_MODEL ** -0.5),
        "router_group_b": nrm(ks[19], (L, N_GROUPS), 0.01),
        "router_expert_w": nrm(ks[20], (L, D_MODEL, N_GROUPS * EXPERTS_PER_GROUP), D_MODEL ** -0.5),
        "router_expert_b": nrm(ks[21], (L, N_GROUPS * EXPERTS_PER_GROUP), 0.01),
        "expert_w_gate": nrm(ks[22], (L, N_GROUPS, EXPERTS_PER_GROUP, D_MODEL, EXPERT_FF), D_MODEL ** -0.5),
        "expert_w_up": nrm(ks[23], (L, N_GROUPS, EXPERTS_PER_GROUP, D_MODEL, EXPERT_FF), D_MODEL ** -0.5),
        "expert_w_down": nrm(ks[24], (L, N_GROUPS, EXPERTS_PER_GROUP, EXPERT_FF, D_MODEL), EXPERT_FF ** -0.5),
    }


def reference(x, mem, mix_norm, w_in, pool_w, pool_scale, q_norm, k_norm, idx_k_norm, w_out,
              xattn_norm, mem_norm, xattn_wq, xattn_wkv, xattn_q_norm, xattn_k_norm, xattn_wo,
              ffn_norm, router_group_w, router_group_b, router_expert_w, router_expert_b,
              expert_w_gate, expert_w_up, expert_w_down):
    B, S, _ = x.shape
    cos_a, sin_a = rope_tables(S, HEAD_DIM)
    cos_i, sin_i = rope_tables(S, IDX_DIM)
    for l in range(DEPTH):
        h = rmsnorm(x, mix_norm[l])
        z = h @ w_in[l]
        u = z[..., OFF_POOL:OFF_Q]
        q = z[..., OFF_Q:OFF_K].reshape(B, S, ATTN_HEADS, HEAD_DIM)
        k = z[..., OFF_K:OFF_V].reshape(B, S, ATTN_HEADS, HEAD_DIM)
        v = z[..., OFF_V:OFF_QI].reshape(B, S, ATTN_HEADS, HEAD_DIM)
        qi = z[..., OFF_QI:OFF_KI].reshape(B, S, IDX_HEADS, IDX_DIM)
        ki = z[..., OFF_KI:OFF_WI]
        wi = z[..., OFF_WI:IN_COLS]
        q = apply_rope(rmsnorm(q, q_norm[l]), cos_a, sin_a)
        k = apply_rope(rmsnorm(k, k_norm[l]), cos_a, sin_a)
        qi = apply_rope(qi, cos_i, sin_i)
        ki = apply_rope(rmsnorm(ki, idx_k_norm[l])[:, :, None, :], cos_i, sin_i)[:, :, 0]
        y_pool = pool_mixer(u, pool_w[l], pool_scale[l])
        y_attn = dsa_attention(q, k, v, qi, ki, wi)
        x = x + jnp.concatenate([y_pool, y_attn], axis=-1) @ w_out[l]
        h = rmsnorm(x, xattn_norm[l])
        x = x + memory_cross_attention(h, mem, mem_norm[l], xattn_wq[l], xattn_wkv[l],
                                       xattn_q_norm[l], xattn_k_norm[l], xattn_wo[l])
        h = rmsnorm(x, ffn_norm[l])
        x = x + hierarchical_moe(h, router_group_w[l], router_group_b[l], router_expert_w[l],
                                 router_expert_b[l], expert_w_gate[l], expert_w_up[l],
                                 expert_w_down[l])
    return x
```

```python
import os
import numpy as np
import ml_dtypes
import concourse.bass as bass
import concourse.mybir as mybir
from concourse.bass_utils import run_bass_kernel_spmd

F32 = mybir.dt.float32
BF16 = mybir.dt.bfloat16
ALU = mybir.AluOpType
AF = mybir.ActivationFunctionType
AX = mybir.AxisListType

S = 4096
D = 1024
NT = 32
NOWN = 16
EPS = 1e-6
TOPK = 256
POOL_WINDOWS = (2, 4, 8, 16)
NEG = -30000.0
BIS_STEPS = 14
N_EXP = 32


class Buf:
    __slots__ = ("name", "w", "r", "excl")

    def __init__(self, name, excl=False):
        self.name = name
        self.w = None
        self.r = []
        self.excl = excl


class Sched:
    ENGS = ("tensor", "vector", "scalar", "gpsimd", "sync")

    def __init__(self, nc, n_dma_sems=6):
        self.nc = nc
        self.prog = {e: [] for e in self.ENGS}
        self.sems = {}
        self.cnt = {}
        self.waited = {e: {} for e in self.ENGS}
        self._ctx = []
        for e in self.ENGS:
            self._mk_sem("E_" + e)
        self.dma_pool = {}
        for q in ("sync", "gpsimd", "scalar"):
            keys = []
            for i in range(n_dma_sems):
                kname = "D_%s%d" % (q, i)
                self._mk_sem(kname)
                keys.append(kname)
            self.dma_pool[q] = [keys, 0]
        self.n_inst = {e: 0 for e in self.ENGS}

    def _mk_sem(self, key):
        cm = self.nc.semaphore(key)
        h = cm.__enter__()
        self._ctx.append(cm)
        self.sems[key] = h
        self.cnt[key] = 0

    def _deps(self, reads, writes, eng=None):
        deps = []
        for b in reads:
            if b.w is not None:
                deps.append(b.w)
            if b.excl:
                own = "E_" + str(eng)
                deps.extend(ev for ev in b.r if ev[0] != own)
        for b in writes:
            if b.w is not None:
                deps.append(b.w)
            deps.extend(b.r)
        return deps

    def _emit_waits(self, eng, deps):
        best = {}
        for (k, v) in deps:
            if v > best.get(k, 0):
                best[k] = v
        wd = self.waited[eng]
        for k, v in best.items():
            if wd.get(k, 0) >= v:
                continue
            wd[k] = v
            self.prog[eng].append(("wait", k, v))

    def _commit(self, ev, reads, writes):
        for b in reads:
            b.r.append(ev)
            if len(b.r) > 24:
                best = {}
                for (k, v) in b.r:
                    if v > best.get(k, 0):
                        best[k] = v
                b.r = list(best.items())
        for b in writes:
            b.w = ev
            b.r = []

    def op(self, eng, fn, reads=(), writes=()):
        pe_mode = None
        if eng == "tensorT":
            eng = "tensor"
            pe_mode = "tr"
        elif eng == "tensor":
            pe_mode = "mm"
        deps = self._deps(reads, writes, eng)
        if eng == "tensor":
            deps = [d_ for d_ in deps if d_[0] != "E_tensor"]
            if pe_mode != getattr(self, "_pe_mode", None) and self.cnt["E_tensor"] > 0:
                deps.append(("E_tensor", self.cnt["E_tensor"]))
            self._pe_mode = pe_mode
        self._emit_waits(eng, deps)
        key = "E_" + eng
        self.cnt[key] += 1
        ev = (key, self.cnt[key])
        self.prog[eng].append(("inst", fn, key, 1))
        self._commit(ev, reads, writes)
        self.n_inst[eng] += 1
        return ev

    def dma(self, q, fn, reads=(), writes=()):
        keys, idx = self.dma_pool[q]
        key = keys[idx % len(keys)]
        self.dma_pool[q][1] = idx + 1
        deps = self._deps(reads, writes)
        if self.cnt[key] > 0:
            deps.append((key, self.cnt[key]))
        self._emit_waits(q, deps)
        self.cnt[key] += 16
        ev = (key, self.cnt[key])
        self.prog[q].append(("inst", fn, key, 16))
        self._commit(ev, reads, writes)
        self.n_inst[q] += 1
        return ev

    def barrier(self):
        for e in self.ENGS:
            deps = [(k, v) for k, v in self.cnt.items() if v > 0 and k != "E_" + e]
            self._emit_waits(e, deps)

    def final_wait(self, eng="sync"):
        deps = [(k, v) for k, v in self.cnt.items() if v > 0 and k.startswith("D_")]
        self._emit_waits(eng, deps)

    def emit(self):
        nc = self.nc
        sems = self.sems
        prog = self.prog

        def run(eng_name):
            items = prog[eng_name]

            def body(e):
                for item in items:
                    if item[0] == "wait":
                        e.wait_ge(sems[item[1]], item[2])
                    else:
                        _, fn, key, n = item
                        fn(e).then_inc(sems[key], n)
            return body

        with nc.Block() as block:
            block.sync(run("sync"))
            block.tensor(run("tensor"))
            block.vector(run("vector"))
            block.scalar(run("scalar"))
            block.gpsimd(run("gpsimd"))
        self.prog = {e: [] for e in self.ENGS}

    def close(self):
        for cm in reversed(self._ctx):
            cm.__exit__(None, None, None)
        self._ctx = []


class Mem:
    def __init__(self, nc):
        self.nc = nc
        self.stack = []

    def sb(self, name, shape, dt):
        cm = self.nc.sbuf_tensor("sb_" + name, list(shape), dt)
        t = cm.__enter__()
        self.stack.append(cm)
        return t

    def ps(self, name, shape, dt):
        cm = self.nc.psum_tensor("pp_" + name, list(shape), dt)
        t = cm.__enter__()
        self.stack.append(cm)
        return t

    def mark(self):
        return len(self.stack)

    def release(self, mark=0):
        while len(self.stack) > mark:
            self.stack.pop().__exit__(None, None, None)


def own_tiles(half):
    out = []
    for j in range(8):
        if half == 0:
            out += [4 * j, 4 * j + 3]
        else:
            out += [4 * j + 1, 4 * j + 2]
    return out


def rope_tables():
    inv = 10000.0 ** (-np.arange(0, 64, 2, dtype=np.float32) / 64)
    ang = np.arange(S, dtype=np.float32)[:, None] * inv[None, :]
    ang = np.concatenate([ang, ang], axis=-1)
    cosT = np.cos(ang).astype(np.float32).T
    sinT = np.sin(ang).astype(np.float32).T
    return np.concatenate([cosT, cosT], 0), np.concatenate([sinT, sinT], 0)


def rot_matrix():
    R = np.zeros((128, 128), np.float32)
    for blk in range(2):
        o = blk * 64
        for dp in range(64):
            if dp < 32:
                R[o + dp + 32, o + dp] = -1.0
            else:
                R[o + dp - 32, o + dp] = 1.0
    return R


def band_full(w, ti, si):
    B = np.zeros((128, 128), np.float32)
    for t in range(128):
        gt = ti * 128 + t
        cnt = min(gt + 1, w)
        for gs in range(max(gt - w + 1, 0), gt + 1):
            if gs // 128 == si:
                B[gs - si * 128, t] += 1.0 / cnt
        if si == ti:
            B[t, t] -= 1.0
    return B


def band_tables(half):
    out = np.zeros((2, 4, 2, 3, 128, 128), np.float32)
    for j0, j in ((0, 0), (1, 1)):
        ot = own_tiles(half)[2 * j:2 * j + 2]
        for g, w in enumerate(POOL_WINDOWS):
            for slot in range(2):
                cands = [4 * j - 1, 4 * j, 4 * j + 1] if slot == 0 else [4 * j + 1, 4 * j + 2, 4 * j + 3]
                for r, si in enumerate(cands):
                    if si < 0:
                        continue
                    out[j0, g, slot, r] = band_full(w, ot[slot], si)
    return out


def adm_tables(half):
    full = np.zeros((128, 128), np.float32)
    none = np.full((128, 128), NEG, np.float32)
    diag = np.zeros((128, 128), np.float32)
    diag[:64, 64:] = NEG
    if half == 0:
        tabs = [[diag, none], [full, diag]]
    else:
        tabs = [[full, diag], [diag, none]]
    return np.array(tabs, np.float32)


def build_program(dbg=False, stop_after=None):
    nc = bass.Bass("TRN2", target_bir_lowering=False)

    in_names = []
    nc._mk_in_names = in_names

    def din(name, shape, dt=F32):
        in_names.append(name)
        return nc.dram_tensor(name, list(shape), dt, kind="ExternalInput").ap()

    def dout(name, shape, dt=F32):
        return nc.dram_tensor(name, list(shape), dt, kind="ExternalOutput").ap()

    USE_A = True
    USE_B = stop_after not in ("A",)
    USE_C = stop_after not in ("A", "B")
    USE_D = stop_after not in ("A", "B", "C")
    x_all = din("x_all", [S, D])
    w_inA = din("w_inA", [D, 1664])
    g1bc_d = din("g1bc", [128, D])
    gvec_d = din("gvec", [128, 8])
    ident_d = din("ident", [128, 128])
    blk1_d = din("blk1", [128, 128])
    ones_d = din("ones", [128, 128])
    rot_d = din("rot", [128, 128])
    cosA_d = din("cosA", [128, S])
    sinA_d = din("sinA", [128, S])
    band_d = din("band", [128, 48, 128])
    poolw_d = din("poolw", [128, 4, 128])
    pscale_d = din("pscale", [128, 4])
    if USE_B:
        x_own = din("x_own", [NOWN * 128, D])
        g1col_d = din("g1col", [128, 8])
        w_inB = din("w_inB", [D, 1032])
        cosO_d = din("cosO", [128, NOWN * 128])
        sinO_d = din("sinO", [128, NOWN * 128])
        adm_d = din("adm", [128, 4, 128])
    if USE_C:
        mem_in = din("mem_b", [256, D])
        g2bc_d = din("g2bc", [128, D])
        gmbc_d = din("gmbc", [128, D])
        w_out_d = din("w_out", [D, D])
        wq_d = din("xwq", [D, 512])
        wkv_d = din("xwkv", [D, 1024])
        wo_d = din("xwo", [512, D])
    if USE_D:
        g3bc_d = din("g3bc", [128, D])
        wr_d = din("wrouter", [D, 36])
        rbias_d = din("rbias", [128, 36])
        wg_d = din("e_wg", [N_EXP, D, 256])
        wu_d = din("e_wu", [N_EXP, D, 256])
        wd_d = din("e_wd", [N_EXP, 256, D])
    out_d = dout("out", [NOWN * 128, D])

    k = Sched(nc)
    m = Mem(nc)

    PS = [m.ps("ps%d" % i, [128, 512], F32) for i in range(7)]
    PSB = [Buf("ps%d" % i, excl=True) for i in range(7)]
    psb = m.ps("psb", [128, 1024], BF16)
    psbB = Buf("psb", excl=True)

    STG_N = 256
    stg = [m.sb("stg%d" % i, [128, STG_N], F32) for i in range(2)]
    stgB = [Buf("stg%d" % i) for i in range(2)]
    stg_cnt = [0]

    stg_set = {"bufs": stg, "B": stgB, "N": STG_N}

    def load_cast(dst_fn, src_fn, n, dstB, eng_cycle=("gpsimd", "vector"), scale_ap=None, scaleB=None):
        lo = 0
        bufs, bB, N_ = stg_set["bufs"], stg_set["B"], stg_set["N"]
        while lo < n:
            hi = min(n, lo + N_)
            i = stg_cnt[0] % len(bufs)
            eng = eng_cycle[stg_cnt[0] % len(eng_cycle)]
            stg_cnt[0] += 1
            k.dma("sync", lambda e, i=i, lo=lo, hi=hi: e.dma_start(out=bufs[i][:, 0:hi - lo], in_=src_fn(lo, hi)), writes=[bB[i]])
            if scale_ap is None:
                k.op(eng, lambda e, i=i, lo=lo, hi=hi: e.tensor_copy(out=dst_fn(lo, hi), in_=bufs[i][:, 0:hi - lo]),
                     reads=[bB[i]], writes=[dstB])
            else:
                k.op("vector", lambda e, i=i, lo=lo, hi=hi: e.tensor_scalar(out=dst_fn(lo, hi), in0=bufs[i][:, 0:hi - lo], scalar1=scale_ap,
                                                                          scalar2=None, op0=ALU.mult),
                     reads=[bB[i], scaleB], writes=[dstB])
            lo = hi

    ident = m.sb("ident", [128, 128], BF16); identB = Buf("ident")
    blk1 = m.sb("blk1", [128, 128], BF16); blk1B = Buf("blk1")
    ones = m.sb("ones", [128, 128], BF16); onesB = Buf("ones")
    rotf = m.sb("rotf", [128, 128], F32); rotfB = Buf("rotf")
    gvec = m.sb("gvec", [128, 8], F32); gvecB = Buf("gvec")
    Rq = m.sb("Rq", [128, 128], BF16); RqB = Buf("Rq")
    Rk = m.sb("Rk", [128, 128], BF16); RkB = Buf("Rk")
    Rki = m.sb("Rki", [128, 128], BF16); RkiB = Buf("Rki")
    Rqi = m.sb("Rqi", [128, 128], BF16); RqiB = Buf("Rqi")
    yT = m.sb("yT", [128, 8, NOWN * 128], BF16)
    yTB = [Buf("yT%d" % i) for i in range(NOWN)]

    load_cast(lambda lo, hi: ident[:, lo:hi], lambda lo, hi: ident_d[:, lo:hi], 128, identB)
    load_cast(lambda lo, hi: blk1[:, lo:hi], lambda lo, hi: blk1_d[:, lo:hi], 128, blk1B)
    load_cast(lambda lo, hi: ones[:, lo:hi], lambda lo, hi: ones_d[:, lo:hi], 128, onesB)
    k.dma("sync", lambda e: e.dma_start(out=rotf[:], in_=rot_d), writes=[rotfB])
    k.dma("sync", lambda e: e.dma_start(out=gvec[:], in_=gvec_d), writes=[gvecB])
    for (Rm, RB, col) in ((Rq, RqB, 0), (Rk, RkB, 1), (Rki, RkiB, 2)):
        k.op("vector", lambda e, Rm=Rm, col=col: e.tensor_scalar(out=Rm[:], in0=rotf[:], scalar1=gvec[:, col:col + 1],
                                                                scalar2=None, op0=ALU.mult),
             reads=[rotfB, gvecB], writes=[RB])
    k.op("vector", lambda e: e.tensor_copy(out=Rqi[:], in_=rotf[:]), reads=[rotfB], writes=[RqiB])

    def norm_transpose(src_ap, gbc, gbcB, xs, xsB, xn, xnB, sqj, sqjB, stat, statB, dst_ap, dstB, extra_reads=()):
        k.op("scalar", lambda e: e.activation(out=sqj[:], in_=src_ap, func=AF.Square, accum_out=stat[:, 0:1]),
             reads=[xsB], writes=[sqjB, statB])
        k.op("scalar", lambda e: e.activation(out=stat[:, 1:2], in_=stat[:, 0:1], func=AF.Sqrt, scale=1.0 / D, bias=EPS),
             reads=[statB], writes=[statB])
        k.op("vector", lambda e: e.reciprocal(out=stat[:, 2:3], in_=stat[:, 1:2]), reads=[statB], writes=[statB])
        if gbc is None:
            k.op("vector", lambda e: e.tensor_scalar(out=xn[:], in0=src_ap, scalar1=stat[:, 2:3], scalar2=None, op0=ALU.mult),
                 reads=[xsB, statB], writes=[xnB])
        else:
            k.op("vector", lambda e: e.scalar_tensor_tensor(out=xn[:], in0=src_ap, scalar=stat[:, 2:3], in1=gbc[:],
                                                            op0=ALU.mult, op1=ALU.mult),
                 reads=[xsB, statB, gbcB], writes=[xnB])
        for kc in range(8):
            k.op("tensorT", lambda e, kc=kc: e.transpose(out=psb[:, kc * 128:(kc + 1) * 128],
                                                       in_=xn[:, kc * 128:(kc + 1) * 128], identity=ident[:]),
                 reads=[xnB, identB], writes=[psbB])
        k.op("scalar", lambda e: e.activation(out=dst_ap, in_=psb[:, :].rearrange("p (a b) -> p a b", a=8), func=AF.Copy),
             reads=[psbB] + list(extra_reads), writes=[dstB])

    class RopeTmp:
        def __init__(self, tag, N):
            self.N = N
            self.zb = m.sb("zb" + tag, [128, N], BF16); self.zbB = Buf("zb" + tag)
            self.sq = m.sb("sq" + tag, [128, N], BF16); self.sqB = Buf("sq" + tag)
            self.t1 = m.sb("t1" + tag, [128, N], F32); self.t1B = Buf("t1" + tag)
            self.t2 = m.sb("t2" + tag, [128, N], F32); self.t2B = Buf("t2" + tag)
            self.rs = m.sb("rs" + tag, [128, N], F32); self.rsB = Buf("rs" + tag)

    def rope_pipeline(zbank, bbank, ssbank, N, Rm, RmB, gcol, norm, cos_ap, sin_ap, csB, out_ap, outB, tmp, hdim=64):
        zT = PS[zbank][:, 0:N]; zB = PSB[zbank]
        bT = PS[bbank][:, 0:N]; bB = PSB[bbank]
        k.op("scalar", lambda e: e.activation(out=tmp.zb[:, 0:N], in_=zT, func=AF.Copy), reads=[zB], writes=[tmp.zbB])
        if norm:
            k.op("scalar", lambda e: e.activation(out=tmp.sq[:, 0:N], in_=zT, func=AF.Square), reads=[zB], writes=[tmp.sqB])
        k.op("tensor", lambda e: e.matmul(bT, lhsT=Rm[:], rhs=tmp.zb[:, 0:N], start=True, stop=True),
             reads=[RmB, tmp.zbB], writes=[bB])
        if norm:
            sT = PS[ssbank][:, 0:N]; sB = PSB[ssbank]
            k.op("tensor", lambda e: e.matmul(sT, lhsT=blk1[:], rhs=tmp.sq[:, 0:N], start=True, stop=True),
                 reads=[blk1B, tmp.sqB], writes=[sB])
        if gcol is not None:
            k.op("vector", lambda e: e.scalar_tensor_tensor(out=tmp.t1[:, 0:N], in0=zT, scalar=gcol, in1=cos_ap,
                                                            op0=ALU.mult, op1=ALU.mult),
                 reads=[zB, gvecB, csB], writes=[tmp.t1B])
        else:
            k.op("vector", lambda e: e.tensor_tensor(out=tmp.t1[:, 0:N], in0=zT, in1=cos_ap, op=ALU.mult),
                 reads=[zB, csB], writes=[tmp.t1B])
        k.op("vector", lambda e: e.tensor_tensor(out=tmp.t2[:, 0:N], in0=bT, in1=sin_ap, op=ALU.mult),
             reads=[bB, csB], writes=[tmp.t2B])
        if norm:
            k.op("scalar", lambda e: e.activation(out=tmp.rs[:, 0:N], in_=sT, func=AF.Sqrt, scale=1.0 / hdim, bias=EPS),
                 reads=[sB], writes=[tmp.rsB])
            k.op("vector", lambda e: e.reciprocal(out=tmp.rs[:, 0:N], in_=tmp.rs[:, 0:N]), reads=[tmp.rsB], writes=[tmp.rsB])
            k.op("gpsimd", lambda e: e.tensor_tensor(out=tmp.t1[:, 0:N], in0=tmp.t1[:, 0:N], in1=tmp.t2[:, 0:N], op=ALU.add),
                 reads=[tmp.t1B, tmp.t2B], writes=[tmp.t1B])
            outs = out_ap if isinstance(out_ap, list) else [(out_ap, 0, 128)]
            for (oap, lo_, hi_) in outs:
                k.op("gpsimd", lambda e, oap=oap, lo_=lo_, hi_=hi_: e.tensor_tensor(out=oap, in0=tmp.t1[lo_:hi_, 0:N], in1=tmp.rs[lo_:hi_, 0:N],
                                                                                 op=ALU.mult),
                     reads=[tmp.t1B, tmp.rsB], writes=[outB])
        else:
            outs = out_ap if isinstance(out_ap, list) else [(out_ap, 0, 128)]
            for (oap, lo_, hi_) in outs:
                k.op("gpsimd", lambda e, oap=oap, lo_=lo_, hi_=hi_: e.tensor_tensor(out=oap, in0=tmp.t1[lo_:hi_, 0:N], in1=tmp.t2[lo_:hi_, 0:N],
                                                                                 op=ALU.add),
                     reads=[tmp.t1B, tmp.t2B], writes=[outB])

    mk_AB = m.mark()
    kT = m.sb("kT", [128, 4, S], BF16)
    kTB = [Buf("kT%d" % j) for j in range(8)]
    V = m.sb("V", [128, NT, 8, 65], BF16)
    VB = [Buf("V%d" % t) for t in range(NT)]
    kiT = m.sb("kiT", [128, S], BF16)
    kiTB = [Buf("kiT%d" % j) for j in range(8)]
    k.op("gpsimd", lambda e: e.memset(V[:, :, :, 64:65], 1.0), writes=VB)

    mk_A = m.mark()
    WA = m.sb("WA", [128, 8, 1664], BF16); WAB = Buf("WA")
    g1bc = m.sb("g1bc", [128, D], F32); g1B = Buf("g1bc")
    xs = [m.sb("xs%d" % i, [128, D], F32) for i in range(2)]; xsB = [Buf("xs%d" % i) for i in range(2)]
    xn = [m.sb("xn%d" % i, [128, D], BF16) for i in range(2)]; xnB = [Buf("xn%d" % i) for i in range(2)]
    sqj = m.sb("sqj", [128, D], BF16); sqjB = Buf("sqj")
    stat = [m.sb("stat%d" % i, [128, 4], F32) for i in range(2)]; statB = [Buf("stat%d" % i) for i in range(2)]
    xnT = m.sb("xnT", [128, 8, 512], BF16); xnTB = [Buf("xnT%d" % t) for t in range(4)]
    csA = [m.sb("csA%d" % i, [128, 512], F32) for i in range(2)]
    snA = [m.sb("snA%d" % i, [128, 512], F32) for i in range(2)]
    csAB = [Buf("csA%d" % i) for i in range(2)]
    tmpA = RopeTmp("A", 512)
    uring = m.sb("uring", [128, 8, 512], BF16); uB = [Buf("u%d" % i) for i in range(8)]
    band = m.sb("band", [128, 48, 128], BF16); bandB = Buf("band")
    poolw = m.sb("poolw", [128, 4, 128], BF16); poolwB = Buf("poolw")
    pscale = m.sb("pscale", [128, 4], F32); pscaleB = Buf("pscale")
    mixsb = m.sb("mixsb", [128, 512], BF16); mixB = Buf("mixsb")

    for kc in range(8):
        load_cast(lambda lo, hi, kc=kc: WA[:, kc, lo:hi], lambda lo, hi, kc=kc: w_inA[kc * 128:(kc + 1) * 128, lo:hi], 1664, WAB)
    k.dma("sync", lambda e: e.dma_start(out=g1bc[:], in_=g1bc_d), writes=[g1B])
    band2 = band[:, :, :].rearrange("p a b -> p (a b)")
    bandd2 = band_d.rearrange("p a b -> p (a b)")
    load_cast(lambda lo, hi: band2[:, lo:hi], lambda lo, hi: bandd2[:, lo:hi], 48 * 128, bandB)
    poolw2 = poolw[:, :, :].rearrange("p a b -> p (a b)")
    poolwd2 = poolw_d.rearrange("p a b -> p (a b)")
    load_cast(lambda lo, hi: poolw2[:, lo:hi], lambda lo, hi: poolwd2[:, lo:hi], 512, poolwB)
    k.dma("sync", lambda e: e.dma_start(out=pscale[:], in_=pscale_d), writes=[pscaleB])

    xcount = 0
    LVL = int(os.environ.get("MK_LVL", "99"))
    NJ = int(os.environ.get("MK_NJ", "8"))
    for j in range(NJ):
        jb = j % 2
        k.dma("sync", lambda e, j=j, jb=jb: e.dma_start(out=csA[jb][:], in_=cosA_d[:, j * 512:(j + 1) * 512]), writes=[csAB[jb]])
        k.dma("sync", lambda e, j=j, jb=jb: e.dma_start(out=snA[jb][:], in_=sinA_d[:, j * 512:(j + 1) * 512]), writes=[csAB[jb]])
        for t in range(4):
            tile = 4 * j + t
            xb = xcount % 2
            xcount += 1
            k.dma("sync", lambda e, tile=tile, xb=xb: e.dma_start(out=xs[xb][:], in_=x_all[tile * 128:(tile + 1) * 128, :]),
                  writes=[xsB[xb]])
            norm_transpose(xs[xb][:], g1bc, g1B, xs[xb], xsB[xb], xn[xb], xnB[xb], sqj, sqjB, stat[xb], statB[xb],
                           xnT[:, :, t * 128:(t + 1) * 128], xnTB[t])
        def tokmajor(t, j=j):
            tile = 4 * j + t
            for kc in range(8):
                k.op("tensor", lambda e, kc=kc: e.matmul(PS[4][:, :], lhsT=xnT[:, kc, t * 128:(t + 1) * 128],
                                                        rhs=WA[:, kc, 0:512], start=(kc == 0), stop=(kc == 7)),
                     reads=[WAB, xnTB[t]], writes=[PSB[4]])
            k.op("scalar", lambda e: e.activation(out=uring[:, tile % 8, :], in_=PS[4][:, :], func=AF.Copy),
                 reads=[PSB[4]], writes=[uB[tile % 8]])
            for kc in range(8):
                k.op("tensor", lambda e, kc=kc: e.matmul(PS[5][:, :], lhsT=xnT[:, kc, t * 128:(t + 1) * 128],
                                                        rhs=WA[:, kc, 512:1024], start=(kc == 0), stop=(kc == 7)),
                     reads=[WAB, xnTB[t]], writes=[PSB[5]])
            k.op("vector", lambda e: e.tensor_copy(out=V[:, tile, :, 0:64], in_=PS[5][:, :].rearrange("p (h d) -> p h d", h=8)),
                 reads=[PSB[5]], writes=[VB[tile]])

        for ci in range(5 if LVL >= 2 else 0):
            colbase = 1024 + ci * 128
            zbank = ci % 2
            for kc in range(8):
                k.op("tensor", lambda e, kc=kc, colbase=colbase, zbank=zbank: e.matmul(
                    PS[zbank][:, :], lhsT=WA[:, kc, colbase:colbase + 128], rhs=xnT[:, kc, :], start=(kc == 0), stop=(kc == 7)),
                    reads=[WAB] + xnTB, writes=[PSB[zbank]])
            if ci < 4 and LVL >= 3:
                tokmajor(ci)
            if ci < 4:
                rope_pipeline(zbank, 2, 3, 512, Rk, RkB, gvec[:, 1:2], True, csA[jb][:], snA[jb][:], csAB[jb],
                              kT[:, ci, j * 512:(j + 1) * 512], kTB[j], tmpA)
            else:
                rope_pipeline(zbank, 2, 3, 512, Rki, RkiB, gvec[:, 2:3], True, csA[jb][:], snA[jb][:], csAB[jb],
                              kiT[:, j * 512:(j + 1) * 512], kiTB[j], tmpA)
        j0 = 0 if j == 0 else 1
        for slot in range(2 if LVL >= 4 else 0):
            cands = [4 * j - 1, 4 * j, 4 * j + 1] if slot == 0 else [4 * j + 1, 4 * j + 2, 4 * j + 3]
            sl = 2 * j + slot
            for g in range(4):
                rs_ = [r for r in range(3) if cands[r] >= 0]
                for r in rs_:
                    bidx = ((j0 * 4 + g) * 2 + slot) * 3 + r
                    k.op("tensor", lambda e, g=g, r=r, bidx=bidx, cand=cands[r], first=(r == rs_[0]), last=(r == rs_[-1]):
                         e.matmul(PS[6][:, g * 128:(g + 1) * 128], lhsT=uring[:, cand % 8, g * 128:(g + 1) * 128],
                                  rhs=band[:, bidx, :], start=first, stop=last),
                         reads=[uB[cands[r] % 8], bandB], writes=[PSB[6]])
            k.op("scalar", lambda e: e.activation(out=mixsb[:], in_=PS[6][:, :], func=AF.Copy), reads=[PSB[6]], writes=[mixB])
            for g in range(4):
                k.op("tensor", lambda e, g=g: e.matmul(PS[6][:, g * 128:(g + 1) * 128], lhsT=poolw[:, g, :],
                                                      rhs=mixsb[:, g * 128:(g + 1) * 128], start=True, stop=True),
                     reads=[poolwB, mixB], writes=[PSB[6]])
            for g in range(4):
                k.op("vector", lambda e, g=g, sl=sl: e.tensor_scalar(out=yT[:, g, sl * 128:(sl + 1) * 128],
                                                                    in0=PS[6][:, g * 128:(g + 1) * 128],
                                                                    scalar1=pscale[:, g:g + 1], scalar2=None, op0=ALU.mult),
                     reads=[PSB[6], pscaleB], writes=[yTB[sl]])

    dbg_outs = {}
    if dbg and stop_after == "A":
        d_kT = dout("d_kT", [128, 4, S], BF16)
        d_V = dout("d_V", [128, NT, 8, 65], BF16)
        d_kiT = dout("d_kiT", [128, S], BF16)
        d_yT = dout("d_yT", [128, 8, NOWN * 128], BF16)
        k.dma("sync", lambda e: e.dma_start(out=d_kT, in_=kT[:]), reads=kTB)
        k.dma("sync", lambda e: e.dma_start(out=d_V, in_=V[:]), reads=VB)
        k.dma("sync", lambda e: e.dma_start(out=d_kiT, in_=kiT[:]), reads=kiTB)
        k.dma("sync", lambda e: e.dma_start(out=d_yT, in_=yT[:]), reads=yTB)

    k.barrier()
    if stop_after == "A":
        k.final_wait("sync")
    k.emit()
    m.release(mk_A)
    if stop_after == "A":
        m.release(0)
        k.close()
        return nc

    WB_ = m.sb("WB", [128, 8, 1032], BF16); WBB = Buf("WB")
    g1col = m.sb("g1col", [128, 8], F32); g1colB = Buf("g1col")
    xs = [m.sb("xsB0", [128, D], F32)]; xsB = [Buf("xsB0")]
    xn = [m.sb("xnB0", [128, D], BF16)]; xnB = [Buf("xnB0")]
    stat = [m.sb("statB%d" % i, [128, 4], F32) for i in range(2)]; statB = [Buf("statB%d" % i) for i in range(2)]
    xoT = m.sb("xoT", [128, 8, 256], BF16); xoTB = [Buf("xoT%d" % i) for i in range(2)]
    csO = m.sb("csO", [128, 256], F32)
    snO = m.sb("snO", [128, 256], F32)
    csOB = Buf("csO")
    tmpB = RopeTmp("B", 256)
    qT = m.sb("qT", [128, 2, 8, 256], BF16); qTB = [Buf("qT%d" % i) for i in range(2)]
    qiT = m.sb("qiT", [128, 8, 256], BF16); qiTB = Buf("qiT")
    k.op("gpsimd", lambda e: e.memset(qT[:], 0.0), writes=qTB)
    k.op("gpsimd", lambda e: e.memset(qiT[:], 0.0), writes=[qiTB])
    wi = m.sb("wi", [128, 2, 8], F32); wiB = Buf("wi")
    Dh = m.sb("Dh", [128, 8, 128], BF16); DhB = Buf("Dh")
    rh = [m.sb("rh%d" % i, [128, 512], BF16) for i in range(2)]; rhB = [Buf("rh%d" % i) for i in range(2)]
    sc2 = [m.sb("scores%d" % i, [128, S], F32) for i in range(2)]; sc2B = [Buf("scores%d" % i) for i in range(2)]
    mb1 = m.sb("mb", [128, S], BF16); mb1B = Buf("mb")
    mb = [mb1, mb1]; mbB = [mb1B, mb1B]
    junk = m.sb("junk", [128, S], mybir.dt.uint8); junkB = Buf("junk")
    adm = m.sb("adm", [128, 4, 128], BF16); admB = Buf("adm")
    ident4 = m.sb("ident4", [128, 512], BF16); ident4B = Buf("ident4")
    PTb = [m.sb("PT%d" % i, [128, 512], BF16) for i in range(2)]; PTB = [Buf("PT%d" % i) for i in range(2)]
    bis = m.sb("bis", [128, 16], F32); bisB = Buf("bis")
    wk = m.sb("wk", [128, BIS_STEPS], F32); wkB = Buf("wk")
    p2 = m.sb("p2", [128, BIS_STEPS], F32); p2B = Buf("p2")
    yat = m.sb("yat", [128, 512], BF16); yatB = Buf("yat")
    rinv = m.sb("rinv", [128, 8], F32); rinvB = Buf("rinv")

    k.dma("sync", lambda e: e.dma_start(out=g1col[:], in_=g1col_d), writes=[g1colB])
    for kc in range(8):
        load_cast(lambda lo, hi, kc=kc: WB_[:, kc, lo:hi], lambda lo, hi, kc=kc: w_inB[kc * 128:(kc + 1) * 128, lo:hi], 1032, WBB,
                  scale_ap=g1col[:, kc:kc + 1], scaleB=g1colB)
    adm2 = adm[:, :, :].rearrange("p a b -> p (a b)")
    admd2 = adm_d.rearrange("p a b -> p (a b)")
    load_cast(lambda lo, hi: adm2[:, lo:hi], lambda lo, hi: admd2[:, lo:hi], 512, admB)
    for s_ in range(BIS_STEPS):
        k.op("gpsimd", lambda e, s_=s_: e.memset(p2[:, s_:s_ + 1], 2.0 ** (-(s_ + 1))), writes=[p2B])
    for c4 in range(4):
        k.op("gpsimd", lambda e, c4=c4: e.tensor_copy(out=ident4[:, c4 * 128:(c4 + 1) * 128], in_=ident[:]), reads=[identB], writes=[ident4B])

    def projB(j):
        jp = j % 2
        k.dma("sync", lambda e: e.dma_start(out=csO[:], in_=cosO_d[:, j * 256:(j + 1) * 256]), writes=[csOB])
        k.dma("sync", lambda e: e.dma_start(out=snO[:], in_=sinO_d[:, j * 256:(j + 1) * 256]), writes=[csOB])
        for slot in range(2):
            sl = 2 * j + slot
            k.dma("sync", lambda e, sl=sl: e.dma_start(out=xs[0][:], in_=x_own[sl * 128:(sl + 1) * 128, :]), writes=[xsB[0]])
            norm_transpose(xs[0][:], None, None, xs[0], xsB[0], xn[0], xnB[0], xn[0], xnB[0], stat[slot], statB[slot],
                           xoT[:, :, slot * 128:(slot + 1) * 128], xoTB[slot])
        for ci in range(8):
            colbase = ci * 128
            zbank = ci % 2
            for kc in range(8):
                k.op("tensor", lambda e, kc=kc, colbase=colbase, zbank=zbank: e.matmul(
                    PS[zbank][:, 0:256], lhsT=WB_[:, kc, colbase:colbase + 128], rhs=xoT[:, kc, :], start=(kc == 0), stop=(kc == 7)),
                    reads=[WBB] + xoTB, writes=[PSB[zbank]])
            if ci < 4:
                rope_pipeline(zbank, 2, 3, 256, Rq, RqB, gvec[:, 0:1], True, csO[:], snO[:], csOB,
                              [(qT[0:64, jp, 2 * ci, :], 0, 64), (qT[64:128, jp, 2 * ci + 1, :], 64, 128)], qTB[jp], tmpB)
            else:
                rope_pipeline(zbank, 2, 3, 256, Rqi, RqiB, None, False, csO[:], snO[:], csOB,
                              [(qiT[0:64, 2 * (ci - 4), :], 0, 64), (qiT[64:128, 2 * (ci - 4) + 1, :], 64, 128)], qiTB, tmpB)
        for slot in range(2):
            for kc in range(8):
                k.op("tensor", lambda e, kc=kc, slot=slot: e.matmul(PS[4][:, slot * 8:(slot + 1) * 8],
                                                                   lhsT=xoT[:, kc, slot * 128:(slot + 1) * 128],
                                                                   rhs=WB_[:, kc, 1024:1032], start=(kc == 0), stop=(kc == 7)),
                     reads=[WBB, xoTB[slot]], writes=[PSB[4]])
        k.op("vector", lambda e: e.tensor_copy(out=wi[:, :, :], in_=PS[4][:, 0:16].rearrange("p (a b) -> p a b", a=2)),
             reads=[PSB[4]], writes=[wiB])

    def nkeys(s_idx):
        j, slot = s_idx // 2, s_idx % 2
        return 4 * j + 2 if slot == 0 else 4 * j + 4

    def idxB(s_idx):
        j, slot = s_idx // 2, s_idx % 2
        n = nkeys(s_idx)
        for h in range(8):
            k.op("gpsimd", lambda e, h=h, slot=slot: e.tensor_scalar(out=Dh[:, h, :], in0=ident[:], scalar1=wi[:, slot, h:h + 1],
                                                                   scalar2=None, op0=ALU.mult),
                 reads=[identB, wiB], writes=[DhB])
        ngroups = (n + 3) // 4
        for kg in range(ngroups):
            ktiles = min(4, n - 4 * kg)
            N = ktiles * 128

            def issue_A(h, kg=kg, N=N, slot=slot):
                ab = h % 2
                k.op("tensor", lambda e: e.matmul(PS[ab][:, 0:N], lhsT=qiT[:, h, slot * 128:(slot + 1) * 128],
                                                  rhs=kiT[:, kg * 512:kg * 512 + N], start=True, stop=True),
                     reads=[qiTB, kiTB[kg]], writes=[PSB[ab]])
                k.op("scalar", lambda e: e.activation(out=rh[ab][:, 0:N], in_=PS[ab][:, 0:N], func=AF.Relu),
                     reads=[PSB[ab]], writes=[rhB[ab]])

            def issue_acc(h, N=N):
                ab = h % 2
                k.op("tensor", lambda e: e.matmul(PS[2][:, 0:N], lhsT=Dh[:, h, :], rhs=rh[ab][:, 0:N], start=(h == 0), stop=False),
                     reads=[DhB, rhB[ab]], writes=[PSB[2]])

            issue_A(0)
            for h in range(8):
                if h + 1 < 8:
                    issue_A(h + 1)
                issue_acc(h)
            if kg == ngroups - 1:
                for r in range(2):
                    p0 = (n - 2 + r) - 4 * kg
                    k.op("tensor", lambda e, r=r, p0=p0, slot=slot: e.matmul(PS[2][:, p0 * 128:(p0 + 1) * 128], lhsT=ident[:],
                                                                             rhs=adm[:, slot * 2 + r, :], start=False, stop=(r == 1)),
                         reads=[identB, admB], writes=[PSB[2]])
            k.op("scalar", lambda e, kg=kg, N=N: e.activation(out=sc2[s_idx % 2][:, kg * 512:kg * 512 + N], in_=PS[2][:, 0:N], func=AF.Copy),
                 reads=[PSB[2]], writes=[sc2B[s_idx % 2]])

    def bisectB(s_idx):
        n = nkeys(s_idx)
        Ntot = n * 128
        scores = sc2[s_idx % 2]; scB = sc2B[s_idx % 2]
        mbuf = mb[s_idx % 2]; mbufB = mbB[s_idx % 2]
        k.op("vector", lambda e: e.tensor_reduce(out=bis[:, 0:1], in_=scores[:, 0:Ntot], axis=AX.X, op=ALU.max),
             reads=[scB], writes=[bisB])
        if n >= 4:
            k.op("vector", lambda e: e.tensor_reduce(out=bis[:, 1:2], in_=scores[:, 0:(n - 2) * 128], axis=AX.X, op=ALU.min),
                 reads=[scB], writes=[bisB])
        else:
            k.op("vector", lambda e: e.memset(bis[:, 1:2], -1024.0), writes=[bisB])
        k.op("vector", lambda e: e.tensor_tensor(out=bis[:, 2:3], in0=bis[:, 0:1], in1=bis[:, 1:2], op=ALU.subtract),
             reads=[bisB], writes=[bisB])
        k.op("vector", lambda e: e.tensor_scalar(out=wk[:, :], in0=p2[:, :], scalar1=bis[:, 2:3], scalar2=None, op0=ALU.mult),
             reads=[bisB, p2B], writes=[wkB])
        k.op("vector", lambda e: e.tensor_tensor(out=bis[:, 3:4], in0=bis[:, 1:2], in1=wk[:, 0:1], op=ALU.add),
             reads=[bisB, wkB], writes=[bisB])
        for s_ in range(BIS_STEPS):
            k.op("vector", lambda e: e.tensor_scalar(out=junk[:, 0:Ntot], in0=scores[:, 0:Ntot], scalar1=bis[:, 3:4],
                                                     scalar2=None, op0=ALU.is_ge, op1=ALU.add, accum_out=bis[:, 4:5]),
                 reads=[scB, bisB], writes=[junkB, bisB])
            if s_ < BIS_STEPS - 1:
                k.op("vector", lambda e: e.tensor_scalar(out=bis[:, 5:6], in0=bis[:, 4:5], scalar1=float(TOPK) - 0.5,
                                                         scalar2=0.5, op0=ALU.is_ge, op1=ALU.subtract),
                     reads=[bisB], writes=[bisB])
                k.op("vector", lambda e, s_=s_: e.scalar_tensor_tensor(out=bis[:, 3:4], in0=bis[:, 5:6], scalar=wk[:, s_:s_ + 1],
                                                                      in1=bis[:, 3:4], op0=ALU.mult, op1=ALU.add),
                     reads=[bisB, wkB], writes=[bisB])
            else:
                k.op("vector", lambda e: e.tensor_scalar(out=bis[:, 5:6], in0=bis[:, 4:5], scalar1=float(TOPK) - 0.5,
                                                         scalar2=1.0, op0=ALU.is_ge, op1=ALU.subtract),
                     reads=[bisB], writes=[bisB])
                k.op("vector", lambda e, s_=s_: e.scalar_tensor_tensor(out=bis[:, 1:2], in0=bis[:, 5:6], scalar=wk[:, s_:s_ + 1],
                                                                      in1=bis[:, 3:4], op0=ALU.mult, op1=ALU.add),
                     reads=[bisB, wkB], writes=[bisB])
        k.op("vector", lambda e: e.tensor_scalar(out=bis[:, 6:7], in0=bis[:, 1:2], scalar1=-10000.0, scalar2=None, op0=ALU.max),
             reads=[bisB], writes=[bisB])
        k.op("vector", lambda e: e.tensor_scalar(out=mbuf[:, 0:Ntot], in0=scores[:, 0:Ntot], scalar1=bis[:, 6:7],
                                                 scalar2=NEG, op0=ALU.is_lt, op1=ALU.mult),
             reads=[scB, bisB], writes=[mbufB])

    def attnB(s_idx):
        j, slot = s_idx // 2, s_idx % 2
        jp = j % 2
        sl = s_idx
        n = nkeys(s_idx)
        mbuf = mb[s_idx % 2]; mbufB = mbB[s_idx % 2]
        U = 2 * n

        def QK(u):
            kt, hg = u // 2, u % 2
            sbk = 3 + hg
            k.op("tensor", lambda e: e.matmul(PS[sbk][:, :], lhsT=mbuf[:, kt * 128:(kt + 1) * 128], rhs=ident4[:, :],
                                              start=True, stop=False), reads=[mbufB, ident4B], writes=[PSB[sbk]])
            for hh in range(4):
                h = hg * 4 + hh
                k.op("tensor", lambda e, h=h, hh=hh: e.matmul(
                    PS[sbk][:, hh * 128:(hh + 1) * 128], lhsT=kT[:, h // 2, kt * 128:(kt + 1) * 128],
                    rhs=qT[:, jp, h, slot * 128:(slot + 1) * 128], start=False, stop=(hh == 3)),
                    reads=[kTB[kt // 4], qTB[jp]], writes=[PSB[sbk]])
            k.op("scalar", lambda e: e.activation(out=PTb[hg][:, :], in_=PS[sbk][:, :], func=AF.Exp, scale=0.125),
                 reads=[PSB[sbk]], writes=[PTB[hg]])

        def PV(u):
            kt, hg = u // 2, u % 2
            for hh in range(4):
                h = hg * 4 + hh
                k.op("tensor", lambda e, h=h, hh=hh: e.matmul(
                    PS[5 + hg][:, hh * 65:(hh + 1) * 65], lhsT=PTb[hg][:, hh * 128:(hh + 1) * 128], rhs=V[:, kt, h, :],
                    start=(kt == 0 and hh == 0), stop=(kt == n - 1), skip_group_check=True),
                    reads=[PTB[hg], VB[kt]], writes=[PSB[5 + hg]])

        QK(0)
        QK(1)
        for u in range(U):
            PV(u)
            if u + 2 < U:
                QK(u + 2)

    def attnB_epi(s_idx):
        sl = s_idx
        for hg in range(2):
            Ov = PS[5 + hg][:, 0:260].rearrange("p (h c) -> p h c", c=65)
            k.op("vector", lambda e, hg=hg, Ov=Ov: e.reciprocal(out=rinv[:, hg * 4:(hg + 1) * 4], in_=Ov[:, :, 64]),
                 reads=[PSB[5 + hg]], writes=[rinvB])
            for hh in range(4):
                h = hg * 4 + hh
                k.op("vector", lambda e, h=h, hh=hh, Ov=Ov: e.tensor_scalar(out=yat[:, h * 64:(h + 1) * 64], in0=Ov[:, hh, 0:64],
                                                                           scalar1=rinv[:, h:h + 1], scalar2=None, op0=ALU.mult),
                     reads=[PSB[5 + hg], rinvB], writes=[yatB])
        for c in range(4):
            k.op("tensorT", lambda e, c=c: e.transpose(out=psb[:, c * 128:(c + 1) * 128], in_=yat[:, c * 128:(c + 1) * 128],
                                                      identity=ident[:]), reads=[yatB, identB], writes=[psbB])
        k.op("scalar", lambda e: e.activation(out=yT[:, 4:8, sl * 128:(sl + 1) * 128],
                                             in_=psb[:, 0:512].rearrange("p (a b) -> p a b", b=128), func=AF.Copy),
             reads=[psbB], writes=[yTB[sl]])

    NSB = 2 * int(os.environ.get("MK_NJB", "8"))
    projB(0)
    idxB(0)
    for s_idx in range(NSB):
        if s_idx + 1 < NSB:
            if (s_idx + 1) % 2 == 0:
                projB((s_idx + 1) // 2)
            idxB(s_idx + 1)
        if s_idx >= 1:
            attnB(s_idx - 1)
        bisectB(s_idx)
        if s_idx >= 1:
            attnB_epi(s_idx - 1)
    attnB(NSB - 1)
    attnB_epi(NSB - 1)

    if dbg and stop_after == "B":
        d_yT = dout("d_yT", [128, 8, NOWN * 128], BF16)
        d_sc = dout("d_sc", [128, S], F32)
        d_mk = dout("d_mk", [128, S], BF16)
        d_bis = dout("d_bis", [128, 16], F32)
        k.dma("sync", lambda e: e.dma_start(out=d_yT, in_=yT[:]), reads=yTB)
        k.dma("sync", lambda e: e.dma_start(out=d_sc, in_=sc2[1][:]), reads=[sc2B[1]])
        k.dma("sync", lambda e: e.dma_start(out=d_mk, in_=mb[1][:]), reads=[mbB[1]])
        k.dma("sync", lambda e: e.dma_start(out=d_bis, in_=bis[:]), reads=[bisB])
    k.barrier()
    if stop_after == "B":
        k.final_wait("sync")
    k.emit()
    m.release(mk_AB)
    if stop_after == "B":
        m.release(0)
        k.close()
        return nc

    xres = m.sb("xres", [128, NOWN, D], F32); xresB = [Buf("xres%d" % i) for i in range(NOWN)]
    g3bc = m.sb("g3bc", [128, D], F32); g3B = Buf("g3bc")
    Wr = m.sb("Wr", [128, 8, 36], BF16); WrB = Buf("Wr")
    rbias = m.sb("rbias", [128, 36], F32); rbB = Buf("rbias")
    h3T = m.sb("h3T", [128, 8, NOWN * 128], BF16); h3TB = [Buf("h3T%d" % i) for i in range(NOWN)]
    gates = m.sb("gates", [128, NOWN, 32], F32); gatesB = [Buf("gates%d" % i) for i in range(NOWN)]
    rt = m.sb("rt", [128, 64], F32); rtB = Buf("rt")
    lgall = m.sb("lgall", [128, NOWN, 36], F32); lgallB = [Buf("lgall%d" % i) for i in range(NOWN)]
    mk_C = m.mark()
    stgC = [m.sb("stgC%d" % i, [128, 512], F32) for i in range(2)]
    stg_set["bufs"] = stgC; stg_set["B"] = [Buf("stgC%d" % i) for i in range(2)]; stg_set["N"] = 512
    k.dma("sync", lambda e: e.dma_start(out=g3bc[:], in_=g3bc_d), writes=[g3B])
    k.dma("sync", lambda e: e.dma_start(out=rbias[:], in_=rbias_d), writes=[rbB])
    for kc in range(8):
        load_cast(lambda lo, hi, kc=kc: Wr[:, kc, lo:hi], lambda lo, hi, kc=kc: wr_d[kc * 128:(kc + 1) * 128, lo:hi], 36, WrB)
    k2T = m.sb("k2T", [128, 4, 256], BF16); k2TB = Buf("k2T")
    V2 = m.sb("V2", [128, 2, 4, 128], BF16); V2B = Buf("V2")
    xn0 = m.sb("xnC0", [128, D], BF16); xn0B = Buf("xnC0")
    xn = [xn0, xn0]; xnB = [xn0B, xn0B]
    sqj = xn0; sqjB = xn0B
    stat = [m.sb("statC%d" % i, [128, 4], F32) for i in range(2)]; statB = [Buf("statC%d" % i) for i in range(2)]
    sqC = m.sb("sqC", [128, 512], BF16); sqCB = Buf("sqC")
    rsC = m.sb("rsC", [128, 512], F32); rsCB = Buf("rsC")

    def qk_norm_fm(zbank, N, gcol, out_ap, outB):
        zT = PS[zbank][:, 0:N]; zB = PSB[zbank]
        k.op("scalar", lambda e: e.activation(out=sqC[:, 0:N], in_=zT, func=AF.Square), reads=[zB], writes=[sqCB])
        k.op("tensor", lambda e: e.matmul(PS[3][:, 0:N], lhsT=ones[:], rhs=sqC[:, 0:N], start=True, stop=True),
             reads=[onesB, sqCB], writes=[PSB[3]])
        k.op("scalar", lambda e: e.activation(out=rsC[:, 0:N], in_=PS[3][:, 0:N], func=AF.Sqrt, scale=1.0 / 128, bias=EPS),
             reads=[PSB[3]], writes=[rsCB])
        k.op("vector", lambda e: e.reciprocal(out=rsC[:, 0:N], in_=rsC[:, 0:N]), reads=[rsCB], writes=[rsCB])
        k.op("vector", lambda e: e.scalar_tensor_tensor(out=out_ap, in0=zT, scalar=gcol, in1=rsC[:, 0:N], op0=ALU.mult, op1=ALU.mult),
             reads=[zB, gvecB, rsCB], writes=[outB])


    mk_M = m.mark()
    Wkv = m.sb("Wkv", [128, 8, D], BF16); WkvB = Buf("Wkv")
    gmbc = m.sb("gmbc", [128, D], F32); gmB = Buf("gmbc")
    memx = [m.sb("memx%d" % i, [128, D], F32) for i in range(2)]; memxB = [Buf("memx%d" % i) for i in range(2)]
    memT = m.sb("memT", [128, 8, 256], BF16); memTB = [Buf("memT%d" % i) for i in range(2)]
    for kc in range(8):
        load_cast(lambda lo, hi, kc=kc: Wkv[:, kc, lo:hi], lambda lo, hi, kc=kc: wkv_d[kc * 128:(kc + 1) * 128, lo:hi], D, WkvB)
    k.dma("sync", lambda e: e.dma_start(out=gmbc[:], in_=gmbc_d), writes=[gmB])
    for mt in range(2):
        k.dma("sync", lambda e, mt=mt: e.dma_start(out=memx[mt][:], in_=mem_in[mt * 128:(mt + 1) * 128, :]), writes=[memxB[mt]])
        norm_transpose(memx[mt][:], gmbc, gmB, memx[mt], memxB[mt], xn[mt], xnB[mt], sqj, sqjB, stat[mt], statB[mt],
                       memT[:, :, mt * 128:(mt + 1) * 128], memTB[mt])
    for h in range(4):
        zb_ = h % 2
        for kc in range(8):
            k.op("tensor", lambda e, kc=kc, h=h, zb_=zb_: e.matmul(PS[zb_][:, 0:256], lhsT=Wkv[:, kc, h * 128:(h + 1) * 128],
                                                                   rhs=memT[:, kc, :], start=(kc == 0), stop=(kc == 7)),
                 reads=[WkvB] + memTB, writes=[PSB[zb_]])
        qk_norm_fm(zb_, 256, gvec[:, 4:5], k2T[:, h, :], k2TB)
    for mt in range(2):
        for kc in range(8):
            k.op("tensor", lambda e, kc=kc, mt=mt: e.matmul(PS[4][:, :], lhsT=memT[:, kc, mt * 128:(mt + 1) * 128],
                                                           rhs=Wkv[:, kc, 512:1024], start=(kc == 0), stop=(kc == 7)),
                 reads=[WkvB, memTB[mt]], writes=[PSB[4]])
        k.op("vector", lambda e, mt=mt: e.tensor_copy(out=V2[:, mt, :, :], in_=PS[4][:, :].rearrange("p (h d) -> p h d", h=4)),
             reads=[PSB[4]], writes=[V2B])
    k.barrier()
    k.emit()
    m.release(mk_M)
    Wout = m.sb("Wout", [128, 8, D], BF16); WoutB = Buf("Wout")
    Wq2 = m.sb("Wq2", [128, 8, 512], BF16); Wq2B = Buf("Wq2")
    Wo2 = m.sb("Wo2", [128, 4, D], BF16); Wo2B = Buf("Wo2")
    g2bc = m.sb("g2bc", [128, D], F32); g2B = Buf("g2bc")
    for kc in range(8):
        load_cast(lambda lo, hi, kc=kc: Wout[:, kc, lo:hi], lambda lo, hi, kc=kc: w_out_d[kc * 128:(kc + 1) * 128, lo:hi], D, WoutB)
        load_cast(lambda lo, hi, kc=kc: Wq2[:, kc, lo:hi], lambda lo, hi, kc=kc: wq_d[kc * 128:(kc + 1) * 128, lo:hi], 512, Wq2B)
    for c in range(4):
        load_cast(lambda lo, hi, c=c: Wo2[:, c, lo:hi], lambda lo, hi, c=c: wo_d[c * 128:(c + 1) * 128, lo:hi], D, Wo2B)
    k.dma("sync", lambda e: e.dma_start(out=g2bc[:], in_=g2bc_d), writes=[g2B])

    h2T = m.sb("h2T", [128, 8, 512], BF16); h2TB = [Buf("h2T%d" % i) for i in range(4)]
    q2n = m.sb("q2n", [128, 512], BF16); q2nB = Buf("q2n")
    E2 = [m.sb("E2_%d" % i, [128, 512], BF16) for i in range(2)]; E2B = [Buf("E2_%d" % i) for i in range(2)]
    o2 = m.sb("o2", [128, 4, 512], BF16); o2B = Buf("o2")
    o2T = m.sb("o2T", [128, 4, 128], BF16); o2TB = Buf("o2T")
    rinv2 = m.sb("rinv2", [128, 4], F32); rinv2B = Buf("rinv2")

    def h3_and_logits(sl):
        xb = sl % 2
        norm_transpose(xres[:, sl, :], g3bc, g3B, None, xresB[sl], xn[xb], xnB[xb], sqj, sqjB, stat[xb], statB[xb],
                       h3T[:, :, sl * 128:(sl + 1) * 128], h3TB[sl])
        for kc in range(8):
            k.op("tensor", lambda e, kc=kc: e.matmul(PS[6][:, 0:36], lhsT=h3T[:, kc, sl * 128:(sl + 1) * 128], rhs=Wr[:, kc, :],
                                                    start=(kc == 0), stop=(kc == 7)), reads=[h3TB[sl], WrB], writes=[PSB[6]])
        k.op("vector", lambda e: e.tensor_tensor(out=lgall[:, sl, :], in0=PS[6][:, 0:36], in1=rbias[:, :], op=ALU.add),
             reads=[PSB[6], rbB], writes=[lgallB[sl]])

    def route(sl):
        lgv = lgall[:, sl, :]

        def V_(eng, fn, reads=(), writes=()):
            k.op(eng, fn, reads=list(reads) + [rtB, lgallB[sl]], writes=list(writes) + [rtB])

        V_("vector", lambda e: e.tensor_reduce(out=rt[:, 0:1], in_=lgv[:, 0:4], axis=AX.X, op=ALU.max))
        V_("vector", lambda e: e.tensor_scalar(out=rt[:, 1:2], in0=rt[:, 0:1], scalar1=-1.0, scalar2=None, op0=ALU.mult))
        V_("vector", lambda e: e.tensor_scalar(out=rt[:, 4:8], in0=lgv[:, 0:4], scalar1=rt[:, 0:1], scalar2=None, op0=ALU.is_ge))
        V_("scalar", lambda e: e.activation(out=rt[:, 8:12], in_=lgv[:, 0:4], func=AF.Exp, bias=rt[:, 1:2], accum_out=rt[:, 2:3]))
        V_("vector", lambda e: e.reciprocal(out=rt[:, 3:4], in_=rt[:, 2:3]))
        V_("vector", lambda e: e.tensor_scalar(out=rt[:, 12:20], in0=lgv[:, 4:12], scalar1=rt[:, 4:5], scalar2=None, op0=ALU.mult))
        for g in range(1, 4):
            V_("vector", lambda e, g=g: e.scalar_tensor_tensor(out=rt[:, 12:20], in0=lgv[:, 4 + 8 * g:12 + 8 * g], scalar=rt[:, 4 + g:5 + g],
                                                              in1=rt[:, 12:20], op0=ALU.mult, op1=ALU.add))
        V_("vector", lambda e: e.tensor_reduce(out=rt[:, 20:21], in_=rt[:, 12:20], axis=AX.X, op=ALU.max))
        V_("vector", lambda e: e.tensor_scalar(out=rt[:, 21:29], in0=rt[:, 12:20], scalar1=rt[:, 20:21], scalar2=None, op0=ALU.is_ge))
        V_("vector", lambda e: e.scalar_tensor_tensor(out=rt[:, 29:37], in0=rt[:, 21:29], scalar=-1e30, in1=rt[:, 12:20],
                                                      op0=ALU.mult, op1=ALU.add))
        V_("vector", lambda e: e.tensor_reduce(out=rt[:, 37:38], in_=rt[:, 29:37], axis=AX.X, op=ALU.max))
        V_("vector", lambda e: e.tensor_scalar(out=rt[:, 38:46], in0=rt[:, 29:37], scalar1=rt[:, 37:38], scalar2=None, op0=ALU.is_ge))
        V_("vector", lambda e: e.tensor_tensor(out=rt[:, 46:47], in0=rt[:, 37:38], in1=rt[:, 20:21], op=ALU.subtract))
        V_("scalar", lambda e: e.activation(out=rt[:, 47:48], in_=rt[:, 46:47], func=AF.Exp))
        V_("vector", lambda e: e.tensor_scalar(out=rt[:, 48:49], in0=rt[:, 47:48], scalar1=1.0, scalar2=None, op0=ALU.add))
        V_("vector", lambda e: e.reciprocal(out=rt[:, 49:50], in_=rt[:, 48:49]))
        V_("vector", lambda e: e.tensor_tensor(out=rt[:, 50:51], in0=rt[:, 47:48], in1=rt[:, 49:50], op=ALU.mult))
        V_("vector", lambda e: e.tensor_tensor(out=rt[:, 51:52], in0=rt[:, 49:50], in1=rt[:, 3:4], op=ALU.mult))
        V_("vector", lambda e: e.tensor_tensor(out=rt[:, 52:53], in0=rt[:, 50:51], in1=rt[:, 3:4], op=ALU.mult))
        V_("vector", lambda e: e.tensor_scalar(out=rt[:, 53:61], in0=rt[:, 21:29], scalar1=rt[:, 51:52], scalar2=None, op0=ALU.mult))
        V_("vector", lambda e: e.scalar_tensor_tensor(out=rt[:, 53:61], in0=rt[:, 38:46], scalar=rt[:, 52:53], in1=rt[:, 53:61],
                                                      op0=ALU.mult, op1=ALU.add))
        for g in range(4):
            k.op("vector", lambda e, g=g, sl=sl: e.tensor_scalar(out=gates[:, sl, g * 8:(g + 1) * 8], in0=rt[:, 53:61],
                                                                scalar1=rt[:, 4 + g:5 + g], scalar2=None, op0=ALU.mult),
                 reads=[rtB], writes=[gatesB[sl]])

    for G in range(4):
        def outproj(s4, G=G):
            sl = 4 * G + s4
            k.dma("sync", lambda e: e.dma_start(out=xres[:, sl, :], in_=x_own[sl * 128:(sl + 1) * 128, :]), writes=[xresB[sl]])
            for half in range(2):
                hb = half
                for kc in range(8):
                    k.op("tensor", lambda e, kc=kc, half=half, hb=hb: e.matmul(
                        PS[hb][:, :], lhsT=yT[:, kc, sl * 128:(sl + 1) * 128], rhs=Wout[:, kc, half * 512:(half + 1) * 512],
                        start=(kc == 0), stop=(kc == 7)), reads=[yTB[sl], WoutB], writes=[PSB[hb]])
                k.op("vector", lambda e, half=half, hb=hb: e.tensor_tensor(
                    out=xres[:, sl, half * 512:(half + 1) * 512], in0=xres[:, sl, half * 512:(half + 1) * 512], in1=PS[hb][:, :], op=ALU.add),
                    reads=[PSB[hb], xresB[sl]], writes=[xresB[sl]])

        def normT(s4, G=G):
            sl = 4 * G + s4
            xb = sl % 2
            norm_transpose(xres[:, sl, :], g2bc, g2B, None, xresB[sl], xn[xb], xnB[xb], sqj, sqjB, stat[xb], statB[xb],
                           h2T[:, :, s4 * 128:(s4 + 1) * 128], h2TB[s4])

        outproj(0)
        for s4 in range(4):
            if s4 + 1 < 4:
                outproj(s4 + 1)
            normT(s4)

        def c_proj(h):
            zb_ = h % 2
            for kc in range(8):
                k.op("tensor", lambda e, kc=kc: e.matmul(PS[zb_][:, :], lhsT=Wq2[:, kc, h * 128:(h + 1) * 128],
                                                        rhs=h2T[:, kc, :], start=(kc == 0), stop=(kc == 7)),
                     reads=[Wq2B] + h2TB, writes=[PSB[zb_]])

        def c_rest(h):
            zb_ = h % 2
            qk_norm_fm(zb_, 512, gvec[:, 3:4], q2n[:, :], q2nB)
            for mt in range(2):
                k.op("tensor", lambda e, mt=mt: e.matmul(PS[4][:, :], lhsT=k2T[:, h, mt * 128:(mt + 1) * 128], rhs=q2n[:, :],
                                                        start=True, stop=True), reads=[k2TB, q2nB], writes=[PSB[4]])
                k.op("scalar", lambda e, mt=mt: e.activation(out=E2[mt][:, :], in_=PS[4][:, :], func=AF.Exp, scale=128.0 ** -0.5),
                     reads=[PSB[4]], writes=[E2B[mt]])
            for tt in range(4):
                for mt in range(2):
                    k.op("tensor", lambda e, tt=tt, mt=mt: e.matmul(PS[5][:, tt * 128:(tt + 1) * 128],
                                                                   lhsT=E2[mt][:, tt * 128:(tt + 1) * 128], rhs=V2[:, mt, h, :],
                                                                   start=(mt == 0), stop=(mt == 1), skip_group_check=True),
                         reads=[E2B[mt], V2B], writes=[PSB[5]])
                for mt in range(2):
                    k.op("tensor", lambda e, tt=tt, mt=mt: e.matmul(PS[6][:, tt:tt + 1], lhsT=E2[mt][:, tt * 128:(tt + 1) * 128],
                                                                   rhs=ones[:, 0:1], start=(mt == 0), stop=(mt == 1),
                                                                   skip_group_check=True),
                         reads=[E2B[mt], onesB], writes=[PSB[6]])
            k.op("vector", lambda e: e.reciprocal(out=rinv2[:, :], in_=PS[6][:, 0:4]), reads=[PSB[6]], writes=[rinv2B])
            for tt in range(4):
                k.op("vector", lambda e, tt=tt: e.tensor_scalar(out=o2[:, tt, h * 128:(h + 1) * 128], in0=PS[5][:, tt * 128:(tt + 1) * 128],
                                                               scalar1=rinv2[:, tt:tt + 1], scalar2=None, op0=ALU.mult),
                     reads=[PSB[5], rinv2B], writes=[o2B])

        c_proj(0)
        for h in range(4):
            if h + 1 < 4:
                c_proj(h + 1)
            c_rest(h)
        for tt in range(4):
            sl = 4 * G + tt
            for c in range(4):
                k.op("tensorT", lambda e, c=c, tt=tt: e.transpose(out=psb[:, c * 128:(c + 1) * 128], in_=o2[:, tt, c * 128:(c + 1) * 128],
                                                                identity=ident[:]), reads=[o2B, identB], writes=[psbB])
            k.op("scalar", lambda e: e.activation(out=o2T[:, :, :], in_=psb[:, 0:512].rearrange("p (a b) -> p a b", b=128), func=AF.Copy),
                 reads=[psbB], writes=[o2TB])
            for half in range(2):
                hb = half
                for c in range(4):
                    k.op("tensor", lambda e, c=c, half=half, hb=hb: e.matmul(PS[hb][:, :], lhsT=o2T[:, c, :],
                                                                             rhs=Wo2[:, c, half * 512:(half + 1) * 512],
                                                                             start=(c == 0), stop=(c == 3)),
                         reads=[o2TB, Wo2B], writes=[PSB[hb]])
                k.op("vector", lambda e, sl=sl, half=half, hb=hb: e.tensor_tensor(
                    out=xres[:, sl, half * 512:(half + 1) * 512], in0=xres[:, sl, half * 512:(half + 1) * 512], in1=PS[hb][:, :], op=ALU.add),
                    reads=[PSB[hb], xresB[sl]], writes=[xresB[sl]])
        if stop_after != "C":
            for tt in range(4):
                h3_and_logits(4 * G + tt)
            for tt in range(4):
                route(4 * G + tt)

    if stop_after == "C":
        for sl in range(NOWN):
            k.dma("sync", lambda e, sl=sl: e.dma_start(out=out_d[sl * 128:(sl + 1) * 128, :], in_=xres[:, sl, :]), reads=[xresB[sl]])
        k.barrier()
        k.final_wait("sync")
        k.emit()
        m.release(0)
        k.close()
        return nc

    k.barrier()
    k.emit()
    m.release(mk_C)
    xn = [m.sb("xnD%d" % i, [128, D], BF16) for i in range(2)]; xnB = [Buf("xnD%d" % i) for i in range(2)]
    sqj = m.sb("sqjD", [128, D], BF16); sqjB = Buf("sqjD")
    stat = [m.sb("statD%d" % i, [128, 4], F32) for i in range(2)]; statB = [Buf("statD%d" % i) for i in range(2)]
    stg_set["bufs"] = stg; stg_set["B"] = stgB; stg_set["N"] = STG_N
    Wgu = [m.sb("Wgu%d" % i, [128, 8, 512], BF16) for i in range(2)]; WguB = [Buf("Wgu%d" % i) for i in range(2)]
    Wd = [m.sb("Wd%d" % i, [128, 2, D], BF16) for i in range(2)]; WdB = [Buf("Wd%d" % i) for i in range(2)]
    est = [m.sb("est%d" % i, [128, 2048], F32) for i in range(3)]; estB = [Buf("est%d" % i) for i in range(3)]
    sa = [m.sb("sa%d" % i, [128, 512], BF16) for i in range(2)]; saB = [Buf("sa%d" % i) for i in range(2)]
    hidT = [m.sb("hidT%d" % i, [128, 2, 512], BF16) for i in range(2)]; hidTB = [Buf("hidT%d" % i) for i in range(2)]
    NE = int(os.environ.get("MK_NE", str(N_EXP)))
    est_i = [0]

    def load_expert(e_):
        eb = e_ % 2
        for which in range(3):
            i = est_i[0] % 3
            est_i[0] += 1
            if which < 2:
                src = (wg_d if which == 0 else wu_d)[e_].rearrange("(kc p) f -> p kc f", p=128)
                k.dma("sync", lambda e, i=i, src=src: e.dma_start(out=est[i][:, :].rearrange("p (kc f) -> p kc f", kc=8), in_=src),
                      writes=[estB[i]])
                k.op("gpsimd", lambda e, i=i, eb=eb, which=which: e.tensor_copy(
                    out=Wgu[eb][:, :, which * 256:(which + 1) * 256], in_=est[i][:, :].rearrange("p (kc f) -> p kc f", kc=8)),
                    reads=[estB[i]], writes=[WguB[eb]])
            else:
                src = wd_d[e_].rearrange("(fc p) d -> p fc d", p=128)
                k.dma("sync", lambda e, i=i, src=src: e.dma_start(out=est[i][:, :].rearrange("p (fc d) -> p fc d", fc=2), in_=src),
                      writes=[estB[i]])
                k.op("gpsimd", lambda e, i=i, eb=eb: e.tensor_copy(out=Wd[eb][:, :, :], in_=est[i][:, :].rearrange("p (fc d) -> p fc d", fc=2)),
                     reads=[estB[i]], writes=[WdB[eb]])

    units = [(e_, G) for e_ in range(NE) for G in range(4)]
    dcount = [0]

    def emit_gu_block(e_, G, ub, fi):
        eb = e_ % 2
        col = (fi // 2) * 256 + (fi % 2) * 128
        for kc in range(8):
            k.op("tensor", lambda e, kc=kc: e.matmul(
                PS[fi][:, :], lhsT=Wgu[eb][:, kc, col:col + 128], rhs=h3T[:, kc, G * 512:(G + 1) * 512],
                start=(kc == 0), stop=(kc == 7)), reads=[WguB[eb]] + h3TB[4 * G:4 * G + 4], writes=[PSB[fi]])
        if fi < 2:
            k.op("scalar", lambda e: e.activation(out=sa[fi][:, :], in_=PS[fi][:, :], func=AF.Silu),
                 reads=[PSB[fi]], writes=[saB[fi]])
        else:
            fc = fi - 2
            k.op("vector", lambda e: e.tensor_tensor(out=hidT[ub][:, fc, :], in0=PS[fi][:, :], in1=sa[fc][:, :], op=ALU.mult),
                 reads=[PSB[fi], saB[fc]], writes=[hidTB[ub]])

    def emit_down_pair(e_, G, ub, t4, half):
        eb = e_ % 2
        tt = 4 * G + t4
        db = 4 + dcount[0] % 3
        dcount[0] += 1
        for fc in range(2):
            k.op("tensor", lambda e, fc=fc: e.matmul(
                PS[db][:, :], lhsT=hidT[ub][:, fc, t4 * 128:(t4 + 1) * 128], rhs=Wd[eb][:, fc, half * 512:(half + 1) * 512],
                start=(fc == 0), stop=(fc == 1)), reads=[hidTB[ub], WdB[eb]], writes=[PSB[db]])
        k.op("vector", lambda e: e.scalar_tensor_tensor(
            out=xres[:, tt, half * 512:(half + 1) * 512], in0=PS[db][:, :], scalar=gates[:, tt, e_:e_ + 1],
            in1=xres[:, tt, half * 512:(half + 1) * 512], op0=ALU.mult, op1=ALU.add),
            reads=[PSB[db], xresB[tt], gatesB[tt]], writes=[xresB[tt]])

    load_expert(0)
    for ui in range(len(units) + 1):
        cur = units[ui] if ui < len(units) else None
        prev = units[ui - 1] if ui >= 1 else None
        for fi in range(4):
            if cur is not None:
                emit_gu_block(cur[0], cur[1], ui % 2, fi)
            if prev is not None:
                for q_ in range(2):
                    pi = 2 * fi + q_
                    emit_down_pair(prev[0], prev[1], (ui - 1) % 2, pi // 2, pi % 2)
        if cur is not None and cur[1] == 0 and cur[0] + 1 < NE:
            load_expert(cur[0] + 1)

    for sl in range(NOWN):
        k.dma("sync", lambda e, sl=sl: e.dma_start(out=out_d[sl * 128:(sl + 1) * 128, :], in_=xres[:, sl, :]), reads=[xresB[sl]])
    k.barrier()
    k.final_wait("sync")
    k.emit()
    m.release(0)
    k.close()
    return nc


def make_in_maps(inputs):
    x = np.asarray(inputs["x"], np.float32)
    mem = np.asarray(inputs["mem"], np.float32)
    w_in = np.asarray(inputs["w_in"], np.float32)[0]
    cosA, sinA = rope_tables()
    w_inA = np.ascontiguousarray(np.concatenate(
        [w_in[:, 0:512], w_in[:, 1536:2048], w_in[:, 1024:1536], w_in[:, 2560:2624], w_in[:, 2560:2624]], axis=1))
    w_inB = np.ascontiguousarray(np.concatenate([w_in[:, 512:1024], w_in[:, 2048:2560], w_in[:, 2624:2632]], axis=1))

    def bc(v):
        return np.ascontiguousarray(np.broadcast_to(np.asarray(v, np.float32).reshape(1, -1), (128, v.size)))

    gvec = np.zeros((128, 8), np.float32)
    gvec[:, 0] = np.tile(np.asarray(inputs["q_norm"], np.float32)[0], 2)
    gvec[:, 1] = np.tile(np.asarray(inputs["k_norm"], np.float32)[0], 2)
    gvec[:, 2] = np.tile(np.asarray(inputs["idx_k_norm"], np.float32)[0], 2)
    gvec[:, 3] = np.asarray(inputs["xattn_q_norm"], np.float32)[0]
    gvec[:, 4] = np.asarray(inputs["xattn_k_norm"], np.float32)[0]
    blk1 = np.zeros((128, 128), np.float32)
    blk1[:64, :64] = 1.0
    blk1[64:, 64:] = 1.0
    poolw = np.ascontiguousarray(np.asarray(inputs["pool_w"], np.float32)[0].transpose(1, 0, 2))
    pscale = np.ascontiguousarray(np.asarray(inputs["pool_scale"], np.float32)[0].reshape(4, 128).T)
    wr = np.ascontiguousarray(np.concatenate([np.asarray(inputs["router_group_w"], np.float32)[0],
                                              np.asarray(inputs["router_expert_w"], np.float32)[0]], axis=1))
    rb = np.concatenate([np.asarray(inputs["router_group_b"], np.float32)[0],
                         np.asarray(inputs["router_expert_b"], np.float32)[0]])
    shared = {
        "w_inA": w_inA, "w_inB": w_inB,
        "g1bc": bc(np.asarray(inputs["mix_norm"])[0]), "g2bc": bc(np.asarray(inputs["xattn_norm"])[0]),
        "g3bc": bc(np.asarray(inputs["ffn_norm"])[0]), "gmbc": bc(np.asarray(inputs["mem_norm"])[0]),
        "g1col": np.ascontiguousarray(np.asarray(inputs["mix_norm"], np.float32)[0].reshape(8, 128).T),
        "gvec": gvec, "ident": np.eye(128, dtype=np.float32), "blk1": blk1, "ones": np.ones((128, 128), np.float32),
        "rot": rot_matrix(), "cosA": cosA, "sinA": sinA, "poolw": poolw, "pscale": pscale,
        "w_out": np.asarray(inputs["w_out"], np.float32)[0], "xwq": np.asarray(inputs["xattn_wq"], np.float32)[0],
        "xwkv": np.asarray(inputs["xattn_wkv"], np.float32)[0], "xwo": np.asarray(inputs["xattn_wo"], np.float32)[0],
        "wrouter": wr, "rbias": bc(rb),
        "e_wg": np.asarray(inputs["expert_w_gate"], np.float32)[0].reshape(N_EXP, D, 256),
        "e_wu": np.asarray(inputs["expert_w_up"], np.float32)[0].reshape(N_EXP, D, 256),
        "e_wd": np.asarray(inputs["expert_w_down"], np.float32)[0].reshape(N_EXP, 256, D),
    }
    per_half = []
    for half in range(2):
        ot = own_tiles(half)
        tok = np.concatenate([np.arange(t * 128, (t + 1) * 128) for t in ot])
        bt = band_tables(half).reshape(48, 128, 128).transpose(1, 0, 2)
        ad = adm_tables(half).reshape(4, 128, 128).transpose(1, 0, 2)
        per_half.append({"tok": tok, "cosO": np.ascontiguousarray(cosA[:, tok]), "sinO": np.ascontiguousarray(sinA[:, tok]),
                         "band": np.ascontiguousarray(bt), "adm": np.ascontiguousarray(ad)})
    in_maps = []
    for c in range(8):
        b, half = c // 2, c % 2
        ph = per_half[half]
        mp = dict(shared)
        mp["x_all"] = np.ascontiguousarray(x[b])
        mp["x_own"] = np.ascontiguousarray(x[b][ph["tok"]])
        mp["mem_b"] = np.ascontiguousarray(mem[b])
        mp["cosO"] = ph["cosO"]; mp["sinO"] = ph["sinO"]; mp["band"] = ph["band"]; mp["adm"] = ph["adm"]
        in_maps.append(mp)
    return in_maps, per_half


_NC_CACHE = {}


def kernel(**inputs):
    in_maps, per_half = make_in_maps(inputs)
    if "nc" not in _NC_CACHE:
        _NC_CACHE["nc"] = build_program()
    nc = _NC_CACHE["nc"]
    in_maps = [{n: mp[n] for n in nc._mk_in_names} for mp in in_maps]
    res = run_bass_kernel_spmd(nc, in_maps, core_ids=list(range(8)))
    out = np.zeros((4, S, D), np.float32)
    for c in range(8):
        b, half = c // 2, c % 2
        out[b][per_half[half]["tok"]] = np.asarray(res.results[c]["out"], np.float32)
    return out
```

```python
import os
import numpy as np
import ml_dtypes
import concourse.bass as bass
import concourse.mybir as mybir
from concourse.bass_utils import run_bass_kernel_spmd

F32 = mybir.dt.float32
BF16 = mybir.dt.bfloat16
ALU = mybir.AluOpType
AF = mybir.ActivationFunctionType
AX = mybir.AxisListType

S = 4096
D = 1024
NT = 32
NOWN = 16
EPS = 1e-6
TOPK = 256
POOL_WINDOWS = (2, 4, 8, 16)
NEG = -30000.0
BIS_STEPS = 14
N_EXP = 32


class Buf:
    __slots__ = ("name", "w", "r", "excl")

    def __init__(self, name, excl=False):
        self.name = name
        self.w = None
        self.r = []
        self.excl = excl


class Sched:
    ENGS = ("tensor", "vector", "scalar", "gpsimd", "sync")

    def __init__(self, nc, n_dma_sems=6):
        self.nc = nc
        self.prog = {e: [] for e in self.ENGS}
        self.sems = {}
        self.cnt = {}
        self.waited = {e: {} for e in self.ENGS}
        self._ctx = []
        for e in self.ENGS:
            self._mk_sem("E_" + e)
        self.dma_pool = {}
        for q in ("sync", "gpsimd", "scalar"):
            keys = []
            for i in range(n_dma_sems):
                kname = "D_%s%d" % (q, i)
                self._mk_sem(kname)
                keys.append(kname)
            self.dma_pool[q] = [keys, 0]
        self.n_inst = {e: 0 for e in self.ENGS}

    def _mk_sem(self, key):
        cm = self.nc.semaphore(key)
        h = cm.__enter__()
        self._ctx.append(cm)
        self.sems[key] = h
        self.cnt[key] = 0

    def _deps(self, reads, writes, eng=None):
        deps = []
        for b in reads:
            if b.w is not None:
                deps.append(b.w)
            if b.excl:
                own = "E_" + str(eng)
                deps.extend(ev for ev in b.r if ev[0] != own)
        for b in writes:
            if b.w is not None:
                deps.append(b.w)
            deps.extend(b.r)
        return deps

    def _emit_waits(self, eng, deps):
        best = {}
        for (k, v) in deps:
            if v > best.get(k, 0):
                best[k] = v
        wd = self.waited[eng]
        for k, v in best.items():
            if wd.get(k, 0) >= v:
                continue
            wd[k] = v
            self.prog[eng].append(("wait", k, v))

    def _commit(self, ev, reads, writes):
        for b in reads:
            b.r.append(ev)
            if len(b.r) > 24:
                best = {}
                for (k, v) in b.r:
                    if v > best.get(k, 0):
                        best[k] = v
                b.r = list(best.items())
        for b in writes:
            b.w = ev
            b.r = []

    def op(self, eng, fn, reads=(), writes=()):
        pe_mode = None
        if eng == "tensorT":
            eng = "tensor"
            pe_mode = "tr"
        elif eng == "tensor":
            pe_mode = "mm"
        deps = self._deps(reads, writes, eng)
        if eng == "tensor":
            deps = [d_ for d_ in deps if d_[0] != "E_tensor"]
            if pe_mode != getattr(self, "_pe_mode", None) and self.cnt["E_tensor"] > 0:
                deps.append(("E_tensor", self.cnt["E_tensor"]))
            self._pe_mode = pe_mode
        self._emit_waits(eng, deps)
        key = "E_" + eng
        self.cnt[key] += 1
        ev = (key, self.cnt[key])
        self.prog[eng].append(("inst", fn, key, 1))
        self._commit(ev, reads, writes)
        self.n_inst[eng] += 1
        return ev

    def dma(self, q, fn, reads=(), writes=()):
        keys, idx = self.dma_pool[q]
        key = keys[idx % len(keys)]
        self.dma_pool[q][1] = idx + 1
        deps = self._deps(reads, writes)
        if self.cnt[key] > 0:
            deps.append((key, self.cnt[key]))
        self._emit_waits(q, deps)
        self.cnt[key] += 16
        ev = (key, self.cnt[key])
        self.prog[q].append(("inst", fn, key, 16))
        self._commit(ev, reads, writes)
        self.n_inst[q] += 1
        return ev

    def barrier(self):
        for e in self.ENGS:
            deps = [(k, v) for k, v in self.cnt.items() if v > 0 and k != "E_" + e]
            self._emit_waits(e, deps)

    def final_wait(self, eng="sync"):
        deps = [(k, v) for k, v in self.cnt.items() if v > 0 and k.startswith("D_")]
        self._emit_waits(eng, deps)

    def emit(self):
        nc = self.nc
        sems = self.sems
        prog = self.prog

        def run(eng_name):
            items = prog[eng_name]

            def body(e):
                for item in items:
                    if item[0] == "wait":
                        e.wait_ge(sems[item[1]], item[2])
                    else:
                        _, fn, key, n = item
                        fn(e).then_inc(sems[key], n)
            return body

        with nc.Block() as block:
            block.sync(run("sync"))
            block.tensor(run("tensor"))
            block.vector(run("vector"))
            block.scalar(run("scalar"))
            block.gpsimd(run("gpsimd"))
        self.prog = {e: [] for e in self.ENGS}

    def close(self):
        for cm in reversed(self._ctx):
            cm.__exit__(None, None, None)
        self._ctx = []


class Mem:
    def __init__(self, nc):
        self.nc = nc
        self.stack = []

    def sb(self, name, shape, dt):
        cm = self.nc.sbuf_tensor("sb_" + name, list(shape), dt)
        t = cm.__enter__()
        self.stack.append(cm)
        return t

    def ps(self, name, shape, dt):
        cm = self.nc.psum_tensor("pp_" + name, list(shape), dt)
        t = cm.__enter__()
        self.stack.append(cm)
        return t

    def mark(self):
        return len(self.stack)

    def release(self, mark=0):
        while len(self.stack) > mark:
            self.stack.pop().__exit__(None, None, None)


def own_tiles(half):
    out = []
    for j in range(8):
        if half == 0:
            out += [4 * j, 4 * j + 3]
        else:
            out += [4 * j + 1, 4 * j + 2]
    return out


def rope_tables():
    inv = 10000.0 ** (-np.arange(0, 64, 2, dtype=np.float32) / 64)
    ang = np.arange(S, dtype=np.float32)[:, None] * inv[None, :]
    ang = np.concatenate([ang, ang], axis=-1)
    cosT = np.cos(ang).astype(np.float32).T
    sinT = np.sin(ang).astype(np.float32).T
    return np.concatenate([cosT, cosT], 0), np.concatenate([sinT, sinT], 0)


def rot_matrix():
    R = np.zeros((128, 128), np.float32)
    for blk in range(2):
        o = blk * 64
        for dp in range(64):
            if dp < 32:
                R[o + dp + 32, o + dp] = -1.0
            else:
                R[o + dp - 32, o + dp] = 1.0
    return R


def band_full(w, ti, si):
    B = np.zeros((128, 128), np.float32)
    for t in range(128):
        gt = ti * 128 + t
        cnt = min(gt + 1, w)
        for gs in range(max(gt - w + 1, 0), gt + 1):
            if gs // 128 == si:
                B[gs - si * 128, t] += 1.0 / cnt
        if si == ti:
            B[t, t] -= 1.0
    return B


def band_tables(half):
    out = np.zeros((2, 4, 2, 3, 128, 128), np.float32)
    for j0, j in ((0, 0), (1, 1)):
        ot = own_tiles(half)[2 * j:2 * j + 2]
        for g, w in enumerate(POOL_WINDOWS):
            for slot in range(2):
                cands = [4 * j - 1, 4 * j, 4 * j + 1] if slot == 0 else [4 * j + 1, 4 * j + 2, 4 * j + 3]
                for r, si in enumerate(cands):
                    if si < 0:
                        continue
                    out[j0, g, slot, r] = band_full(w, ot[slot], si)
    return out


def adm_tables(half):
    full = np.zeros((128, 128), np.float32)
    none = np.full((128, 128), NEG, np.float32)
    diag = np.zeros((128, 128), np.float32)
    diag[:64, 64:] = NEG
    if half == 0:
        tabs = [[diag, none], [full, diag]]
    else:
        tabs = [[full, diag], [diag, none]]
    return np.array(tabs, np.float32)


def build_program(dbg=False, stop_after=None):
    nc = bass.Bass("TRN2", target_bir_lowering=False)

    in_names = []
    nc._mk_in_names = in_names

    def din(name, shape, dt=F32):
        in_names.append(name)
        return nc.dram_tensor(name, list(shape), dt, kind="ExternalInput").ap()

    def dout(name, shape, dt=F32):
        return nc.dram_tensor(name, list(shape), dt, kind="ExternalOutput").ap()

    USE_A = True
    USE_B = stop_after not in ("A",)
    USE_C = stop_after not in ("A", "B")
    USE_D = stop_after not in ("A", "B", "C")
    x_all = din("x_all", [S, D])
    w_inA = din("w_inA", [D, 1664])
    g1bc_d = din("g1bc", [128, D])
    gvec_d = din("gvec", [128, 8])
    ident_d = din("ident", [128, 128])
    blk1_d = din("blk1", [128, 128])
    ones_d = din("ones", [128, 128])
    rot_d = din("rot", [128, 128])
    cosA_d = din("cosA", [128, S])
    sinA_d = din("sinA", [128, S])
    band_d = din("band", [128, 48, 128])
    poolw_d = din("poolw", [128, 4, 128])
    pscale_d = din("pscale", [128, 4])
    if USE_B:
        x_own = din("x_own", [NOWN * 128, D])
        g1col_d = din("g1col", [128, 8])
        w_inB = din("w_inB", [D, 1032])
        cosO_d = din("cosO", [128, NOWN * 128])
        sinO_d = din("sinO", [128, NOWN * 128])
        adm_d = din("adm", [128, 4, 128])
    if USE_C:
        mem_in = din("mem_b", [256, D])
        g2bc_d = din("g2bc", [128, D])
        gmbc_d = din("gmbc", [128, D])
        w_out_d = din("w_out", [D, D])
        wq_d = din("xwq", [D, 512])
        wkv_d = din("xwkv", [D, 1024])
        wo_d = din("xwo", [512, D])
    if USE_D:
        g3bc_d = din("g3bc", [128, D])
        wr_d = din("wrouter", [D, 36])
        rbias_d = din("rbias", [128, 36])
        wg_d = din("e_wg", [N_EXP, D, 256])
        wu_d = din("e_wu", [N_EXP, D, 256])
        wd_d = din("e_wd", [N_EXP, 256, D])
    out_d = dout("out", [NOWN * 128, D])

    k = Sched(nc)
    m = Mem(nc)

    PS = [m.ps("ps%d" % i, [128, 512], F32) for i in range(7)]
    PSB = [Buf("ps%d" % i, excl=True) for i in range(7)]
    psb = m.ps("psb", [128, 1024], BF16)
    psbB = Buf("psb", excl=True)

    STG_N = 256
    stg = [m.sb("stg%d" % i, [128, STG_N], F32) for i in range(3)]
    stgB = [Buf("stg%d" % i) for i in range(3)]
    stg_cnt = [0]

    stg_set = {"bufs": stg, "B": stgB, "N": STG_N}

    def load_cast(dst_fn, src_fn, n, dstB, eng_cycle=("gpsimd", "vector"), scale_ap=None, scaleB=None):
        lo = 0
        bufs, bB, N_ = stg_set["bufs"], stg_set["B"], stg_set["N"]
        while lo < n:
            hi = min(n, lo + N_)
            i = stg_cnt[0] % len(bufs)
            eng = eng_cycle[stg_cnt[0] % len(eng_cycle)]
            stg_cnt[0] += 1
            k.dma("sync", lambda e, i=i, lo=lo, hi=hi: e.dma_start(out=bufs[i][:, 0:hi - lo], in_=src_fn(lo, hi)), writes=[bB[i]])
            if scale_ap is None:
                k.op(eng, lambda e, i=i, lo=lo, hi=hi: e.tensor_copy(out=dst_fn(lo, hi), in_=bufs[i][:, 0:hi - lo]),
                     reads=[bB[i]], writes=[dstB])
            else:
                k.op("vector", lambda e, i=i, lo=lo, hi=hi: e.tensor_scalar(out=dst_fn(lo, hi), in0=bufs[i][:, 0:hi - lo], scalar1=scale_ap,
                                                                          scalar2=None, op0=ALU.mult),
                     reads=[bB[i], scaleB], writes=[dstB])
            lo = hi

    ident = m.sb("ident", [128, 128], BF16); identB = Buf("ident")
    blk1 = m.sb("blk1", [128, 128], BF16); blk1B = Buf("blk1")
    ones = m.sb("ones", [128, 128], BF16); onesB = Buf("ones")
    rotf = m.sb("rotf", [128, 128], F32); rotfB = Buf("rotf")
    gvec = m.sb("gvec", [128, 8], F32); gvecB = Buf("gvec")
    Rq = m.sb("Rq", [128, 128], BF16); RqB = Buf("Rq")
    Rk = m.sb("Rk", [128, 128], BF16); RkB = Buf("Rk")
    Rki = m.sb("Rki", [128, 128], BF16); RkiB = Buf("Rki")
    Rqi = m.sb("Rqi", [128, 128], BF16); RqiB = Buf("Rqi")
    yT = m.sb("yT", [128, 8, NOWN * 128], BF16)
    yTB = [Buf("yT%d" % i) for i in range(NOWN)]

    load_cast(lambda lo, hi: ident[:, lo:hi], lambda lo, hi: ident_d[:, lo:hi], 128, identB)
    load_cast(lambda lo, hi: blk1[:, lo:hi], lambda lo, hi: blk1_d[:, lo:hi], 128, blk1B)
    load_cast(lambda lo, hi: ones[:, lo:hi], lambda lo, hi: ones_d[:, lo:hi], 128, onesB)
    k.dma("sync", lambda e: e.dma_start(out=rotf[:], in_=rot_d), writes=[rotfB])
    k.dma("sync", lambda e: e.dma_start(out=gvec[:], in_=gvec_d), writes=[gvecB])
    for (Rm, RB, col) in ((Rq, RqB, 0), (Rk, RkB, 1), (Rki, RkiB, 2)):
        k.op("vector", lambda e, Rm=Rm, col=col: e.tensor_scalar(out=Rm[:], in0=rotf[:], scalar1=gvec[:, col:col + 1],
                                                                scalar2=None, op0=ALU.mult),
             reads=[rotfB, gvecB], writes=[RB])
    k.op("vector", lambda e: e.tensor_copy(out=Rqi[:], in_=rotf[:]), reads=[rotfB], writes=[RqiB])

    def norm_transpose(src_ap, gbc, gbcB, xs, xsB, xn, xnB, sqj, sqjB, stat, statB, dst_ap, dstB, extra_reads=()):
        k.op("scalar", lambda e: e.activation(out=sqj[:], in_=src_ap, func=AF.Square, accum_out=stat[:, 0:1]),
             reads=[xsB], writes=[sqjB, statB])
        k.op("scalar", lambda e: e.activation(out=stat[:, 1:2], in_=stat[:, 0:1], func=AF.Sqrt, scale=1.0 / D, bias=EPS),
             reads=[statB], writes=[statB])
        k.op("vector", lambda e: e.reciprocal(out=stat[:, 2:3], in_=stat[:, 1:2]), reads=[statB], writes=[statB])
        if gbc is None:
            k.op("vector", lambda e: e.tensor_scalar(out=xn[:], in0=src_ap, scalar1=stat[:, 2:3], scalar2=None, op0=ALU.mult),
                 reads=[xsB, statB], writes=[xnB])
        else:
            k.op("vector", lambda e: e.scalar_tensor_tensor(out=xn[:], in0=src_ap, scalar=stat[:, 2:3], in1=gbc[:],
                                                            op0=ALU.mult, op1=ALU.mult),
                 reads=[xsB, statB, gbcB], writes=[xnB])
        for kc in range(8):
            k.op("tensorT", lambda e, kc=kc: e.transpose(out=psb[:, kc * 128:(kc + 1) * 128],
                                                       in_=xn[:, kc * 128:(kc + 1) * 128], identity=ident[:]),
                 reads=[xnB, identB], writes=[psbB])
        k.op("scalar", lambda e: e.activation(out=dst_ap, in_=psb[:, :].rearrange("p (a b) -> p a b", a=8), func=AF.Copy),
             reads=[psbB] + list(extra_reads), writes=[dstB])

    class RopeTmp:
        def __init__(self, tag, N):
            self.N = N
            self.zb = m.sb("zb" + tag, [128, N], BF16); self.zbB = Buf("zb" + tag)
            self.sq = m.sb("sq" + tag, [128, N], BF16); self.sqB = Buf("sq" + tag)
            self.t1 = m.sb("t1" + tag, [128, N], F32); self.t1B = Buf("t1" + tag)
            self.t2 = m.sb("t2" + tag, [128, N], F32); self.t2B = Buf("t2" + tag)
            self.rs = m.sb("rs" + tag, [128, N], F32); self.rsB = Buf("rs" + tag)

    def rope_pipeline(zbank, bbank, ssbank, N, Rm, RmB, gcol, norm, cos_ap, sin_ap, csB, out_ap, outB, tmp, hdim=64):
        zT = PS[zbank][:, 0:N]; zB = PSB[zbank]
        bT = PS[bbank][:, 0:N]; bB = PSB[bbank]
        k.op("scalar", lambda e: e.activation(out=tmp.zb[:, 0:N], in_=zT, func=AF.Copy), reads=[zB], writes=[tmp.zbB])
        if norm:
            k.op("scalar", lambda e: e.activation(out=tmp.sq[:, 0:N], in_=zT, func=AF.Square), reads=[zB], writes=[tmp.sqB])
        k.op("tensor", lambda e: e.matmul(bT, lhsT=Rm[:], rhs=tmp.zb[:, 0:N], start=True, stop=True),
             reads=[RmB, tmp.zbB], writes=[bB])
        if norm:
            sT = PS[ssbank][:, 0:N]; sB = PSB[ssbank]
            k.op("tensor", lambda e: e.matmul(sT, lhsT=blk1[:], rhs=tmp.sq[:, 0:N], start=True, stop=True),
                 reads=[blk1B, tmp.sqB], writes=[sB])
        if gcol is not None:
            k.op("vector", lambda e: e.scalar_tensor_tensor(out=tmp.t1[:, 0:N], in0=zT, scalar=gcol, in1=cos_ap,
                                                            op0=ALU.mult, op1=ALU.mult),
                 reads=[zB, gvecB, csB], writes=[tmp.t1B])
        else:
            k.op("vector", lambda e: e.tensor_tensor(out=tmp.t1[:, 0:N], in0=zT, in1=cos_ap, op=ALU.mult),
                 reads=[zB, csB], writes=[tmp.t1B])
        k.op("vector", lambda e: e.tensor_tensor(out=tmp.t2[:, 0:N], in0=bT, in1=sin_ap, op=ALU.mult),
             reads=[bB, csB], writes=[tmp.t2B])
        if norm:
            k.op("scalar", lambda e: e.activation(out=tmp.rs[:, 0:N], in_=sT, func=AF.Sqrt, scale=1.0 / hdim, bias=EPS),
                 reads=[sB], writes=[tmp.rsB])
            k.op("vector", lambda e: e.reciprocal(out=tmp.rs[:, 0:N], in_=tmp.rs[:, 0:N]), reads=[tmp.rsB], writes=[tmp.rsB])
            k.op("gpsimd", lambda e: e.tensor_tensor(out=tmp.t1[:, 0:N], in0=tmp.t1[:, 0:N], in1=tmp.t2[:, 0:N], op=ALU.add),
                 reads=[tmp.t1B, tmp.t2B], writes=[tmp.t1B])
            outs = out_ap if isinstance(out_ap, list) else [(out_ap, 0, 128)]
            for (oap, lo_, hi_) in outs:
                k.op("gpsimd", lambda e, oap=oap, lo_=lo_, hi_=hi_: e.tensor_tensor(out=oap, in0=tmp.t1[lo_:hi_, 0:N], in1=tmp.rs[lo_:hi_, 0:N],
                                                                                 op=ALU.mult),
                     reads=[tmp.t1B, tmp.rsB], writes=[outB])
        else:
            outs = out_ap if isinstance(out_ap, list) else [(out_ap, 0, 128)]
            for (oap, lo_, hi_) in outs:
                k.op("gpsimd", lambda e, oap=oap, lo_=lo_, hi_=hi_: e.tensor_tensor(out=oap, in0=tmp.t1[lo_:hi_, 0:N], in1=tmp.t2[lo_:hi_, 0:N],
                                                                                 op=ALU.add),
                     reads=[tmp.t1B, tmp.t2B], writes=[outB])

    mk_AB = m.mark()
    kT = m.sb("kT", [128, 4, S], BF16)
    kTB = [Buf("kT%d" % j) for j in range(8)]
    V = m.sb("V", [128, NT, 8, 65], BF16)
    VB = [Buf("V%d" % t) for t in range(NT)]
    kiT = m.sb("kiT", [128, S], BF16)
    kiTB = [Buf("kiT%d" % j) for j in range(8)]
    k.op("gpsimd", lambda e: e.memset(V[:, :, :, 64:65], 1.0), writes=VB)

    mk_A = m.mark()
    WA = m.sb("WA", [128, 8, 1664], BF16); WAB = Buf("WA")
    g1bc = m.sb("g1bc", [128, D], F32); g1B = Buf("g1bc")
    xs = [m.sb("xs%d" % i, [128, D], F32) for i in range(2)]; xsB = [Buf("xs%d" % i) for i in range(2)]
    xn = [m.sb("xn%d" % i, [128, D], BF16) for i in range(2)]; xnB = [Buf("xn%d" % i) for i in range(2)]
    sqj = m.sb("sqj", [128, D], BF16); sqjB = Buf("sqj")
    stat = [m.sb("stat%d" % i, [128, 4], F32) for i in range(2)]; statB = [Buf("stat%d" % i) for i in range(2)]
    xnT = m.sb("xnT", [128, 8, 512], BF16); xnTB = [Buf("xnT%d" % t) for t in range(4)]
    csA = [m.sb("csA%d" % i, [128, 512], F32) for i in range(2)]
    snA = [m.sb("snA%d" % i, [128, 512], F32) for i in range(2)]
    csAB = [Buf("csA%d" % i) for i in range(2)]
    tmpA = RopeTmp("A", 512)
    uring = m.sb("uring", [128, 8, 512], BF16); uB = [Buf("u%d" % i) for i in range(8)]
    band = m.sb("band", [128, 48, 128], BF16); bandB = Buf("band")
    poolw = m.sb("poolw", [128, 4, 128], BF16); poolwB = Buf("poolw")
    pscale = m.sb("pscale", [128, 4], F32); pscaleB = Buf("pscale")
    mixsb = m.sb("mixsb", [128, 512], BF16); mixB = Buf("mixsb")

    for kc in range(8):
        load_cast(lambda lo, hi, kc=kc: WA[:, kc, lo:hi], lambda lo, hi, kc=kc: w_inA[kc * 128:(kc + 1) * 128, lo:hi], 1664, WAB)
    k.dma("sync", lambda e: e.dma_start(out=g1bc[:], in_=g1bc_d), writes=[g1B])
    band2 = band[:, :, :].rearrange("p a b -> p (a b)")
    bandd2 = band_d.rearrange("p a b -> p (a b)")
    load_cast(lambda lo, hi: band2[:, lo:hi], lambda lo, hi: bandd2[:, lo:hi], 48 * 128, bandB)
    poolw2 = poolw[:, :, :].rearrange("p a b -> p (a b)")
    poolwd2 = poolw_d.rearrange("p a b -> p (a b)")
    load_cast(lambda lo, hi: poolw2[:, lo:hi], lambda lo, hi: poolwd2[:, lo:hi], 512, poolwB)
    k.dma("sync", lambda e: e.dma_start(out=pscale[:], in_=pscale_d), writes=[pscaleB])

    xcount = 0
    LVL = int(os.environ.get("MK_LVL", "99"))
    NJ = int(os.environ.get("MK_NJ", "8"))
    for j in range(NJ):
        jb = j % 2
        k.dma("sync", lambda e, j=j, jb=jb: e.dma_start(out=csA[jb][:], in_=cosA_d[:, j * 512:(j + 1) * 512]), writes=[csAB[jb]])
        k.dma("sync", lambda e, j=j, jb=jb: e.dma_start(out=snA[jb][:], in_=sinA_d[:, j * 512:(j + 1) * 512]), writes=[csAB[jb]])
        for t in range(4):
            tile = 4 * j + t
            xb = xcount % 2
            xcount += 1
            k.dma("sync", lambda e, tile=tile, xb=xb: e.dma_start(out=xs[xb][:], in_=x_all[tile * 128:(tile + 1) * 128, :]),
                  writes=[xsB[xb]])
            norm_transpose(xs[xb][:], g1bc, g1B, xs[xb], xsB[xb], xn[xb], xnB[xb], sqj, sqjB, stat[xb], statB[xb],
                           xnT[:, :, t * 128:(t + 1) * 128], xnTB[t])
        def tokmajor(t, j=j):
            tile = 4 * j + t
            for kc in range(8):
                k.op("tensor", lambda e, kc=kc: e.matmul(PS[4][:, :], lhsT=xnT[:, kc, t * 128:(t + 1) * 128],
                                                        rhs=WA[:, kc, 0:512], start=(kc == 0), stop=(kc == 7)),
                     reads=[WAB, xnTB[t]], writes=[PSB[4]])
            k.op("scalar", lambda e: e.activation(out=uring[:, tile % 8, :], in_=PS[4][:, :], func=AF.Copy),
                 reads=[PSB[4]], writes=[uB[tile % 8]])
            for kc in range(8):
                k.op("tensor", lambda e, kc=kc: e.matmul(PS[5][:, :], lhsT=xnT[:, kc, t * 128:(t + 1) * 128],
                                                        rhs=WA[:, kc, 512:1024], start=(kc == 0), stop=(kc == 7)),
                     reads=[WAB, xnTB[t]], writes=[PSB[5]])
            k.op("vector", lambda e: e.tensor_copy(out=V[:, tile, :, 0:64], in_=PS[5][:, :].rearrange("p (h d) -> p h d", h=8)),
                 reads=[PSB[5]], writes=[VB[tile]])

        for ci in range(5 if LVL >= 2 else 0):
            colbase = 1024 + ci * 128
            zbank = ci % 2
            for kc in range(8):
                k.op("tensor", lambda e, kc=kc, colbase=colbase, zbank=zbank: e.matmul(
                    PS[zbank][:, :], lhsT=WA[:, kc, colbase:colbase + 128], rhs=xnT[:, kc, :], start=(kc == 0), stop=(kc == 7)),
                    reads=[WAB] + xnTB, writes=[PSB[zbank]])
            if ci < 4 and LVL >= 3:
                tokmajor(ci)
            if ci < 4:
                rope_pipeline(zbank, 2, 3, 512, Rk, RkB, gvec[:, 1:2], True, csA[jb][:], snA[jb][:], csAB[jb],
                              kT[:, ci, j * 512:(j + 1) * 512], kTB[j], tmpA)
            else:
                rope_pipeline(zbank, 2, 3, 512, Rki, RkiB, gvec[:, 2:3], True, csA[jb][:], snA[jb][:], csAB[jb],
                              kiT[:, j * 512:(j + 1) * 512], kiTB[j], tmpA)
        j0 = 0 if j == 0 else 1
        for slot in range(2 if LVL >= 4 else 0):
            cands = [4 * j - 1, 4 * j, 4 * j + 1] if slot == 0 else [4 * j + 1, 4 * j + 2, 4 * j + 3]
            sl = 2 * j + slot
            for g in range(4):
                rs_ = [r for r in range(3) if cands[r] >= 0]
                for r in rs_:
                    bidx = ((j0 * 4 + g) * 2 + slot) * 3 + r
                    k.op("tensor", lambda e, g=g, r=r, bidx=bidx, cand=cands[r], first=(r == rs_[0]), last=(r == rs_[-1]):
                         e.matmul(PS[6][:, g * 128:(g + 1) * 128], lhsT=uring[:, cand % 8, g * 128:(g + 1) * 128],
                                  rhs=band[:, bidx, :], start=first, stop=last),
                         reads=[uB[cands[r] % 8], bandB], writes=[PSB[6]])
            k.op("scalar", lambda e: e.activation(out=mixsb[:], in_=PS[6][:, :], func=AF.Copy), reads=[PSB[6]], writes=[mixB])
            for g in range(4):
                k.op("tensor", lambda e, g=g: e.matmul(PS[6][:, g * 128:(g + 1) * 128], lhsT=poolw[:, g, :],
                                                      rhs=mixsb[:, g * 128:(g + 1) * 128], start=True, stop=True),
                     reads=[poolwB, mixB], writes=[PSB[6]])
            for g in range(4):
                k.op("vector", lambda e, g=g, sl=sl: e.tensor_scalar(out=yT[:, g, sl * 128:(sl + 1) * 128],
                                                                    in0=PS[6][:, g * 128:(g + 1) * 128],
                                                                    scalar1=pscale[:, g:g + 1], scalar2=None, op0=ALU.mult),
                     reads=[PSB[6], pscaleB], writes=[yTB[sl]])

    dbg_outs = {}
    if dbg and stop_after == "A":
        d_kT = dout("d_kT", [128, 4, S], BF16)
        d_V = dout("d_V", [128, NT, 8, 65], BF16)
        d_kiT = dout("d_kiT", [128, S], BF16)
        d_yT = dout("d_yT", [128, 8, NOWN * 128], BF16)
        k.dma("sync", lambda e: e.dma_start(out=d_kT, in_=kT[:]), reads=kTB)
        k.dma("sync", lambda e: e.dma_start(out=d_V, in_=V[:]), reads=VB)
        k.dma("sync", lambda e: e.dma_start(out=d_kiT, in_=kiT[:]), reads=kiTB)
        k.dma("sync", lambda e: e.dma_start(out=d_yT, in_=yT[:]), reads=yTB)

    k.barrier()
    if stop_after == "A":
        k.final_wait("sync")
    k.emit()
    m.release(mk_A)
    if stop_after == "A":
        m.release(0)
        k.close()
        return nc

    WB_ = m.sb("WB", [128, 8, 1032], BF16); WBB = Buf("WB")
    g1col = m.sb("g1col", [128, 8], F32); g1colB = Buf("g1col")
    xs = [m.sb("xsB0", [128, D], F32)]; xsB = [Buf("xsB0")]
    xn = [m.sb("xnB0", [128, D], BF16)]; xnB = [Buf("xnB0")]
    stat = [m.sb("statB%d" % i, [128, 4], F32) for i in range(2)]; statB = [Buf("statB%d" % i) for i in range(2)]
    xoT = m.sb("xoT", [128, 8, 256], BF16); xoTB = [Buf("xoT%d" % i) for i in range(2)]
    csO = m.sb("csO", [128, 256], F32)
    snO = m.sb("snO", [128, 256], F32)
    csOB = Buf("csO")
    tmpB = RopeTmp("B", 256)
    qT = m.sb("qT", [128, 2, 8, 256], BF16); qTB = [Buf("qT%d" % i) for i in range(2)]
    qiT = m.sb("qiT", [128, 8, 256], BF16); qiTB = Buf("qiT")
    k.op("gpsimd", lambda e: e.memset(qT[:], 0.0), writes=qTB)
    k.op("gpsimd", lambda e: e.memset(qiT[:], 0.0), writes=[qiTB])
    wi = m.sb("wi", [128, 2, 8], F32); wiB = Buf("wi")
    Dh = m.sb("Dh", [128, 8, 128], BF16); DhB = Buf("Dh")
    rh = [m.sb("rh%d" % i, [128, 512], BF16) for i in range(2)]; rhB = [Buf("rh%d" % i) for i in range(2)]
    sc2 = [m.sb("scores%d" % i, [128, S], F32) for i in range(2)]; sc2B = [Buf("scores%d" % i) for i in range(2)]
    mb1 = m.sb("mb", [128, S], BF16); mb1B = Buf("mb")
    mb = [mb1, mb1]; mbB = [mb1B, mb1B]
    junk = m.sb("junk", [128, S], mybir.dt.uint8); junkB = Buf("junk")
    adm = m.sb("adm", [128, 4, 128], BF16); admB = Buf("adm")
    ident4 = m.sb("ident4", [128, 512], BF16); ident4B = Buf("ident4")
    PTb = [m.sb("PT%d" % i, [128, 512], BF16) for i in range(2)]; PTB = [Buf("PT%d" % i) for i in range(2)]
    bis = m.sb("bis", [128, 16], F32); bisB = Buf("bis")
    wk = m.sb("wk", [128, BIS_STEPS], F32); wkB = Buf("wk")
    p2 = m.sb("p2", [128, BIS_STEPS], F32); p2B = Buf("p2")
    yat = m.sb("yat", [128, 512], BF16); yatB = Buf("yat")
    rinv = m.sb("rinv", [128, 8], F32); rinvB = Buf("rinv")

    k.dma("sync", lambda e: e.dma_start(out=g1col[:], in_=g1col_d), writes=[g1colB])
    for kc in range(8):
        load_cast(lambda lo, hi, kc=kc: WB_[:, kc, lo:hi], lambda lo, hi, kc=kc: w_inB[kc * 128:(kc + 1) * 128, lo:hi], 1032, WBB,
                  scale_ap=g1col[:, kc:kc + 1], scaleB=g1colB)
    adm2 = adm[:, :, :].rearrange("p a b -> p (a b)")
    admd2 = adm_d.rearrange("p a b -> p (a b)")
    load_cast(lambda lo, hi: adm2[:, lo:hi], lambda lo, hi: admd2[:, lo:hi], 512, admB)
    for s_ in range(BIS_STEPS):
        k.op("gpsimd", lambda e, s_=s_: e.memset(p2[:, s_:s_ + 1], 2.0 ** (-(s_ + 1))), writes=[p2B])
    for c4 in range(4):
        k.op("gpsimd", lambda e, c4=c4: e.tensor_copy(out=ident4[:, c4 * 128:(c4 + 1) * 128], in_=ident[:]), reads=[identB], writes=[ident4B])

    def projB(j):
        jp = j % 2
        k.dma("sync", lambda e: e.dma_start(out=csO[:], in_=cosO_d[:, j * 256:(j + 1) * 256]), writes=[csOB])
        k.dma("sync", lambda e: e.dma_start(out=snO[:], in_=sinO_d[:, j * 256:(j + 1) * 256]), writes=[csOB])
        for slot in range(2):
            sl = 2 * j + slot
            k.dma("sync", lambda e, sl=sl: e.dma_start(out=xs[0][:], in_=x_own[sl * 128:(sl + 1) * 128, :]), writes=[xsB[0]])
            norm_transpose(xs[0][:], None, None, xs[0], xsB[0], xn[0], xnB[0], xn[0], xnB[0], stat[slot], statB[slot],
                           xoT[:, :, slot * 128:(slot + 1) * 128], xoTB[slot])
        for ci in range(8):
            colbase = ci * 128
            zbank = ci % 2
            for kc in range(8):
                k.op("tensor", lambda e, kc=kc, colbase=colbase, zbank=zbank: e.matmul(
                    PS[zbank][:, 0:256], lhsT=WB_[:, kc, colbase:colbase + 128], rhs=xoT[:, kc, :], start=(kc == 0), stop=(kc == 7)),
                    reads=[WBB] + xoTB, writes=[PSB[zbank]])
            if ci < 4:
                rope_pipeline(zbank, 2, 3, 256, Rq, RqB, gvec[:, 0:1], True, csO[:], snO[:], csOB,
                              [(qT[0:64, jp, 2 * ci, :], 0, 64), (qT[64:128, jp, 2 * ci + 1, :], 64, 128)], qTB[jp], tmpB)
            else:
                rope_pipeline(zbank, 2, 3, 256, Rqi, RqiB, None, False, csO[:], snO[:], csOB,
                              [(qiT[0:64, 2 * (ci - 4), :], 0, 64), (qiT[64:128, 2 * (ci - 4) + 1, :], 64, 128)], qiTB, tmpB)
        for slot in range(2):
            for kc in range(8):
                k.op("tensor", lambda e, kc=kc, slot=slot: e.matmul(PS[4][:, slot * 8:(slot + 1) * 8],
                                                                   lhsT=xoT[:, kc, slot * 128:(slot + 1) * 128],
                                                                   rhs=WB_[:, kc, 1024:1032], start=(kc == 0), stop=(kc == 7)),
                     reads=[WBB, xoTB[slot]], writes=[PSB[4]])
        k.op("vector", lambda e: e.tensor_copy(out=wi[:, :, :], in_=PS[4][:, 0:16].rearrange("p (a b) -> p a b", a=2)),
             reads=[PSB[4]], writes=[wiB])

    def nkeys(s_idx):
        j, slot = s_idx // 2, s_idx % 2
        return 4 * j + 2 if slot == 0 else 4 * j + 4

    def idxB(s_idx):
        j, slot = s_idx // 2, s_idx % 2
        n = nkeys(s_idx)
        for h in range(8):
            k.op("gpsimd", lambda e, h=h, slot=slot: e.tensor_scalar(out=Dh[:, h, :], in0=ident[:], scalar1=wi[:, slot, h:h + 1],
                                                                   scalar2=None, op0=ALU.mult),
                 reads=[identB, wiB], writes=[DhB])
        ngroups = (n + 3) // 4
        for kg in range(ngroups):
            ktiles = min(4, n - 4 * kg)
            N = ktiles * 128

            def issue_A(h, kg=kg, N=N, slot=slot):
                ab = h % 2
                k.op("tensor", lambda e: e.matmul(PS[ab][:, 0:N], lhsT=qiT[:, h, slot * 128:(slot + 1) * 128],
                                                  rhs=kiT[:, kg * 512:kg * 512 + N], start=True, stop=True),
                     reads=[qiTB, kiTB[kg]], writes=[PSB[ab]])
                k.op("scalar", lambda e: e.activation(out=rh[ab][:, 0:N], in_=PS[ab][:, 0:N], func=AF.Relu),
                     reads=[PSB[ab]], writes=[rhB[ab]])

            def issue_acc(h, N=N):
                ab = h % 2
                k.op("tensor", lambda e: e.matmul(PS[2][:, 0:N], lhsT=Dh[:, h, :], rhs=rh[ab][:, 0:N], start=(h == 0), stop=False),
                     reads=[DhB, rhB[ab]], writes=[PSB[2]])

            issue_A(0)
            for h in range(8):
                if h + 1 < 8:
                    issue_A(h + 1)
                issue_acc(h)
            if kg == ngroups - 1:
                for r in range(2):
                    p0 = (n - 2 + r) - 4 * kg
                    k.op("tensor", lambda e, r=r, p0=p0, slot=slot: e.matmul(PS[2][:, p0 * 128:(p0 + 1) * 128], lhsT=ident[:],
                                                                             rhs=adm[:, slot * 2 + r, :], start=False, stop=(r == 1)),
                         reads=[identB, admB], writes=[PSB[2]])
            k.op("scalar", lambda e, kg=kg, N=N: e.activation(out=sc2[s_idx % 2][:, kg * 512:kg * 512 + N], in_=PS[2][:, 0:N], func=AF.Copy),
                 reads=[PSB[2]], writes=[sc2B[s_idx % 2]])

    def bisectB(s_idx):
        n = nkeys(s_idx)
        Ntot = n * 128
        scores = sc2[s_idx % 2]; scB = sc2B[s_idx % 2]
        mbuf = mb[s_idx % 2]; mbufB = mbB[s_idx % 2]
        k.op("vector", lambda e: e.tensor_reduce(out=bis[:, 0:1], in_=scores[:, 0:Ntot], axis=AX.X, op=ALU.max),
             reads=[scB], writes=[bisB])
        if n >= 4:
            k.op("vector", lambda e: e.tensor_reduce(out=bis[:, 1:2], in_=scores[:, 0:(n - 2) * 128], axis=AX.X, op=ALU.min),
                 reads=[scB], writes=[bisB])
        else:
            k.op("vector", lambda e: e.memset(bis[:, 1:2], -1024.0), writes=[bisB])
        k.op("vector", lambda e: e.tensor_tensor(out=bis[:, 2:3], in0=bis[:, 0:1], in1=bis[:, 1:2], op=ALU.subtract),
             reads=[bisB], writes=[bisB])
        k.op("vector", lambda e: e.tensor_scalar(out=wk[:, :], in0=p2[:, :], scalar1=bis[:, 2:3], scalar2=None, op0=ALU.mult),
             reads=[bisB, p2B], writes=[wkB])
        k.op("vector", lambda e: e.tensor_tensor(out=bis[:, 3:4], in0=bis[:, 1:2], in1=wk[:, 0:1], op=ALU.add),
             reads=[bisB, wkB], writes=[bisB])
        for s_ in range(BIS_STEPS):
            k.op("vector", lambda e: e.tensor_scalar(out=junk[:, 0:Ntot], in0=scores[:, 0:Ntot], scalar1=bis[:, 3:4],
                                                     scalar2=None, op0=ALU.is_ge, op1=ALU.add, accum_out=bis[:, 4:5]),
                 reads=[scB, bisB], writes=[junkB, bisB])
            if s_ < BIS_STEPS - 1:
                k.op("vector", lambda e: e.tensor_scalar(out=bis[:, 5:6], in0=bis[:, 4:5], scalar1=float(TOPK) - 0.5,
                                                         scalar2=0.5, op0=ALU.is_ge, op1=ALU.subtract),
                     reads=[bisB], writes=[bisB])
                k.op("vector", lambda e, s_=s_: e.scalar_tensor_tensor(out=bis[:, 3:4], in0=bis[:, 5:6], scalar=wk[:, s_:s_ + 1],
                                                                      in1=bis[:, 3:4], op0=ALU.mult, op1=ALU.add),
                     reads=[bisB, wkB], writes=[bisB])
            else:
                k.op("vector", lambda e: e.tensor_scalar(out=bis[:, 5:6], in0=bis[:, 4:5], scalar1=float(TOPK) - 0.5,
                                                         scalar2=1.0, op0=ALU.is_ge, op1=ALU.subtract),
                     reads=[bisB], writes=[bisB])
                k.op("vector", lambda e, s_=s_: e.scalar_tensor_tensor(out=bis[:, 1:2], in0=bis[:, 5:6], scalar=wk[:, s_:s_ + 1],
                                                                      in1=bis[:, 3:4], op0=ALU.mult, op1=ALU.add),
                     reads=[bisB, wkB], writes=[bisB])
        k.op("vector", lambda e: e.tensor_scalar(out=bis[:, 6:7], in0=bis[:, 1:2], scalar1=-10000.0, scalar2=None, op0=ALU.max),
             reads=[bisB], writes=[bisB])
        k.op("vector", lambda e: e.tensor_scalar(out=mbuf[:, 0:Ntot], in0=scores[:, 0:Ntot], scalar1=bis[:, 6:7],
                                                 scalar2=NEG, op0=ALU.is_lt, op1=ALU.mult),
             reads=[scB, bisB], writes=[mbufB])

    def attnB(s_idx):
        j, slot = s_idx // 2, s_idx % 2
        jp = j % 2
        sl = s_idx
        n = nkeys(s_idx)
        mbuf = mb[s_idx % 2]; mbufB = mbB[s_idx % 2]
        U = 2 * n

        def QK(u):
            kt, hg = u // 2, u % 2
            sbk = 3 + hg
            k.op("tensor", lambda e: e.matmul(PS[sbk][:, :], lhsT=mbuf[:, kt * 128:(kt + 1) * 128], rhs=ident4[:, :],
                                              start=True, stop=False), reads=[mbufB, ident4B], writes=[PSB[sbk]])
            for hh in range(4):
                h = hg * 4 + hh
                k.op("tensor", lambda e, h=h, hh=hh: e.matmul(
                    PS[sbk][:, hh * 128:(hh + 1) * 128], lhsT=kT[:, h // 2, kt * 128:(kt + 1) * 128],
                    rhs=qT[:, jp, h, slot * 128:(slot + 1) * 128], start=False, stop=(hh == 3)),
                    reads=[kTB[kt // 4], qTB[jp]], writes=[PSB[sbk]])
            k.op("scalar", lambda e: e.activation(out=PTb[hg][:, :], in_=PS[sbk][:, :], func=AF.Exp, scale=0.125),
                 reads=[PSB[sbk]], writes=[PTB[hg]])

        def PV(u):
            kt, hg = u // 2, u % 2
            for hh in range(4):
                h = hg * 4 + hh
                k.op("tensor", lambda e, h=h, hh=hh: e.matmul(
                    PS[5 + hg][:, hh * 65:(hh + 1) * 65], lhsT=PTb[hg][:, hh * 128:(hh + 1) * 128], rhs=V[:, kt, h, :],
                    start=(kt == 0 and hh == 0), stop=(kt == n - 1), skip_group_check=True),
                    reads=[PTB[hg], VB[kt]], writes=[PSB[5 + hg]])

        QK(0)
        QK(1)
        for u in range(U):
            PV(u)
            if u + 2 < U:
                QK(u + 2)

    def attnB_epi(s_idx):
        sl = s_idx
        for hg in range(2):
            Ov = PS[5 + hg][:, 0:260].rearrange("p (h c) -> p h c", c=65)
            k.op("vector", lambda e, hg=hg, Ov=Ov: e.reciprocal(out=rinv[:, hg * 4:(hg + 1) * 4], in_=Ov[:, :, 64]),
                 reads=[PSB[5 + hg]], writes=[rinvB])
            for hh in range(4):
                h = hg * 4 + hh
                k.op("vector", lambda e, h=h, hh=hh, Ov=Ov: e.tensor_scalar(out=yat[:, h * 64:(h + 1) * 64], in0=Ov[:, hh, 0:64],
                                                                           scalar1=rinv[:, h:h + 1], scalar2=None, op0=ALU.mult),
                     reads=[PSB[5 + hg], rinvB], writes=[yatB])
        for c in range(4):
            k.op("tensorT", lambda e, c=c: e.transpose(out=psb[:, c * 128:(c + 1) * 128], in_=yat[:, c * 128:(c + 1) * 128],
                                                      identity=ident[:]), reads=[yatB, identB], writes=[psbB])
        k.op("scalar", lambda e: e.activation(out=yT[:, 4:8, sl * 128:(sl + 1) * 128],
                                             in_=psb[:, 0:512].rearrange("p (a b) -> p a b", b=128), func=AF.Copy),
             reads=[psbB], writes=[yTB[sl]])

    NSB = 2 * int(os.environ.get("MK_NJB", "8"))
    projB(0)
    idxB(0)
    for s_idx in range(NSB):
        if s_idx + 1 < NSB:
            if (s_idx + 1) % 2 == 0:
                projB((s_idx + 1) // 2)
            idxB(s_idx + 1)
        if s_idx >= 1:
            attnB(s_idx - 1)
        bisectB(s_idx)
        if s_idx >= 1:
            attnB_epi(s_idx - 1)
    attnB(NSB - 1)
    attnB_epi(NSB - 1)

    if dbg and stop_after == "B":
        d_yT = dout("d_yT", [128, 8, NOWN * 128], BF16)
        d_sc = dout("d_sc", [128, S], F32)
        d_mk = dout("d_mk", [128, S], BF16)
        d_bis = dout("d_bis", [128, 16], F32)
        k.dma("sync", lambda e: e.dma_start(out=d_yT, in_=yT[:]), reads=yTB)
        k.dma("sync", lambda e: e.dma_start(out=d_sc, in_=sc2[1][:]), reads=[sc2B[1]])
        k.dma("sync", lambda e: e.dma_start(out=d_mk, in_=mb[1][:]), reads=[mbB[1]])
        k.dma("sync", lambda e: e.dma_start(out=d_bis, in_=bis[:]), reads=[bisB])
    k.barrier()
    if stop_after == "B":
        k.final_wait("sync")
    k.emit()
    m.release(mk_AB)
    if stop_after == "B":
        m.release(0)
        k.close()
        return nc

    xres = m.sb("xres", [128, NOWN, D], F32); xresB = [Buf("xres%d" % i) for i in range(NOWN)]
    mk_C = m.mark()
    stgC = [m.sb("stgC%d" % i, [128, 1024], F32) for i in range(4)]
    stg_set["bufs"] = stgC; stg_set["B"] = [Buf("stgC%d" % i) for i in range(4)]; stg_set["N"] = 1024
    Wout = m.sb("Wout", [128, 8, D], BF16); WoutB = Buf("Wout")
    Wq2 = m.sb("Wq2", [128, 8, 512], BF16); Wq2B = Buf("Wq2")
    Wo2 = m.sb("Wo2", [128, 4, D], BF16); Wo2B = Buf("Wo2")
    g2bc = m.sb("g2bc", [128, D], F32); g2B = Buf("g2bc")
    k2T = m.sb("k2T", [128, 4, 256], BF16); k2TB = Buf("k2T")
    V2 = m.sb("V2", [128, 2, 4, 128], BF16); V2B = Buf("V2")
    xn = [m.sb("xnC%d" % i, [128, D], BF16) for i in range(2)]; xnB = [Buf("xnC%d" % i) for i in range(2)]
    sqj = m.sb("sqjC", [128, D], BF16); sqjB = Buf("sqjC")
    stat = [m.sb("statC%d" % i, [128, 4], F32) for i in range(2)]; statB = [Buf("statC%d" % i) for i in range(2)]
    sqC = m.sb("sqC", [128, 512], BF16); sqCB = Buf("sqC")
    rsC = m.sb("rsC", [128, 512], F32); rsCB = Buf("rsC")

    def qk_norm_fm(zbank, N, gcol, out_ap, outB):
        zT = PS[zbank][:, 0:N]; zB = PSB[zbank]
        k.op("scalar", lambda e: e.activation(out=sqC[:, 0:N], in_=zT, func=AF.Square), reads=[zB], writes=[sqCB])
        k.op("tensor", lambda e: e.matmul(PS[3][:, 0:N], lhsT=ones[:], rhs=sqC[:, 0:N], start=True, stop=True),
             reads=[onesB, sqCB], writes=[PSB[3]])
        k.op("scalar", lambda e: e.activation(out=rsC[:, 0:N], in_=PS[3][:, 0:N], func=AF.Sqrt, scale=1.0 / 128, bias=EPS),
             reads=[PSB[3]], writes=[rsCB])
        k.op("vector", lambda e: e.reciprocal(out=rsC[:, 0:N], in_=rsC[:, 0:N]), reads=[rsCB], writes=[rsCB])
        k.op("vector", lambda e: e.scalar_tensor_tensor(out=out_ap, in0=zT, scalar=gcol, in1=rsC[:, 0:N], op0=ALU.mult, op1=ALU.mult),
             reads=[zB, gvecB, rsCB], writes=[outB])

    for kc in range(8):
        load_cast(lambda lo, hi, kc=kc: Wout[:, kc, lo:hi], lambda lo, hi, kc=kc: w_out_d[kc * 128:(kc + 1) * 128, lo:hi], D, WoutB)
        load_cast(lambda lo, hi, kc=kc: Wq2[:, kc, lo:hi], lambda lo, hi, kc=kc: wq_d[kc * 128:(kc + 1) * 128, lo:hi], 512, Wq2B)
    for c in range(4):
        load_cast(lambda lo, hi, c=c: Wo2[:, c, lo:hi], lambda lo, hi, c=c: wo_d[c * 128:(c + 1) * 128, lo:hi], D, Wo2B)
    k.dma("sync", lambda e: e.dma_start(out=g2bc[:], in_=g2bc_d), writes=[g2B])

    mk_M = m.mark()
    Wkv = m.sb("Wkv", [128, 8, D], BF16); WkvB = Buf("Wkv")
    gmbc = m.sb("gmbc", [128, D], F32); gmB = Buf("gmbc")
    memx = [m.sb("memx%d" % i, [128, D], F32) for i in range(2)]; memxB = [Buf("memx%d" % i) for i in range(2)]
    memT = m.sb("memT", [128, 8, 256], BF16); memTB = [Buf("memT%d" % i) for i in range(2)]
    for kc in range(8):
        load_cast(lambda lo, hi, kc=kc: Wkv[:, kc, lo:hi], lambda lo, hi, kc=kc: wkv_d[kc * 128:(kc + 1) * 128, lo:hi], D, WkvB)
    k.dma("sync", lambda e: e.dma_start(out=gmbc[:], in_=gmbc_d), writes=[gmB])
    for mt in range(2):
        k.dma("sync", lambda e, mt=mt: e.dma_start(out=memx[mt][:], in_=mem_in[mt * 128:(mt + 1) * 128, :]), writes=[memxB[mt]])
        norm_transpose(memx[mt][:], gmbc, gmB, memx[mt], memxB[mt], xn[mt], xnB[mt], sqj, sqjB, stat[mt], statB[mt],
                       memT[:, :, mt * 128:(mt + 1) * 128], memTB[mt])
    for h in range(4):
        zb_ = h % 2
        for kc in range(8):
            k.op("tensor", lambda e, kc=kc, h=h, zb_=zb_: e.matmul(PS[zb_][:, 0:256], lhsT=Wkv[:, kc, h * 128:(h + 1) * 128],
                                                                   rhs=memT[:, kc, :], start=(kc == 0), stop=(kc == 7)),
                 reads=[WkvB] + memTB, writes=[PSB[zb_]])
        qk_norm_fm(zb_, 256, gvec[:, 4:5], k2T[:, h, :], k2TB)
    for mt in range(2):
        for kc in range(8):
            k.op("tensor", lambda e, kc=kc, mt=mt: e.matmul(PS[4][:, :], lhsT=memT[:, kc, mt * 128:(mt + 1) * 128],
                                                           rhs=Wkv[:, kc, 512:1024], start=(kc == 0), stop=(kc == 7)),
                 reads=[WkvB, memTB[mt]], writes=[PSB[4]])
        k.op("vector", lambda e, mt=mt: e.tensor_copy(out=V2[:, mt, :, :], in_=PS[4][:, :].rearrange("p (h d) -> p h d", h=4)),
             reads=[PSB[4]], writes=[V2B])
    k.barrier()
    k.emit()
    m.release(mk_M)

    h2T = m.sb("h2T", [128, 8, 512], BF16); h2TB = [Buf("h2T%d" % i) for i in range(4)]
    q2n = m.sb("q2n", [128, 512], BF16); q2nB = Buf("q2n")
    E2 = [m.sb("E2_%d" % i, [128, 512], BF16) for i in range(2)]; E2B = [Buf("E2_%d" % i) for i in range(2)]
    o2 = m.sb("o2", [128, 4, 512], BF16); o2B = Buf("o2")
    o2T = m.sb("o2T", [128, 4, 128], BF16); o2TB = Buf("o2T")
    rinv2 = m.sb("rinv2", [128, 4], F32); rinv2B = Buf("rinv2")

    for G in range(4):
        def outproj(s4, G=G):
            sl = 4 * G + s4
            k.dma("sync", lambda e: e.dma_start(out=xres[:, sl, :], in_=x_own[sl * 128:(sl + 1) * 128, :]), writes=[xresB[sl]])
            for half in range(2):
                hb = half
                for kc in range(8):
                    k.op("tensor", lambda e, kc=kc, half=half, hb=hb: e.matmul(
                        PS[hb][:, :], lhsT=yT[:, kc, sl * 128:(sl + 1) * 128], rhs=Wout[:, kc, half * 512:(half + 1) * 512],
                        start=(kc == 0), stop=(kc == 7)), reads=[yTB[sl], WoutB], writes=[PSB[hb]])
                k.op("vector", lambda e, half=half, hb=hb: e.tensor_tensor(
                    out=xres[:, sl, half * 512:(half + 1) * 512], in0=xres[:, sl, half * 512:(half + 1) * 512], in1=PS[hb][:, :], op=ALU.add),
                    reads=[PSB[hb], xresB[sl]], writes=[xresB[sl]])

        def normT(s4, G=G):
            sl = 4 * G + s4
            xb = sl % 2
            norm_transpose(xres[:, sl, :], g2bc, g2B, None, xresB[sl], xn[xb], xnB[xb], sqj, sqjB, stat[xb], statB[xb],
                           h2T[:, :, s4 * 128:(s4 + 1) * 128], h2TB[s4])

        outproj(0)
        for s4 in range(4):
            if s4 + 1 < 4:
                outproj(s4 + 1)
            normT(s4)

        def c_proj(h):
            zb_ = h % 2
            for kc in range(8):
                k.op("tensor", lambda e, kc=kc: e.matmul(PS[zb_][:, :], lhsT=Wq2[:, kc, h * 128:(h + 1) * 128],
                                                        rhs=h2T[:, kc, :], start=(kc == 0), stop=(kc == 7)),
                     reads=[Wq2B] + h2TB, writes=[PSB[zb_]])

        def c_rest(h):
            zb_ = h % 2
            qk_norm_fm(zb_, 512, gvec[:, 3:4], q2n[:, :], q2nB)
            for mt in range(2):
                k.op("tensor", lambda e, mt=mt: e.matmul(PS[4][:, :], lhsT=k2T[:, h, mt * 128:(mt + 1) * 128], rhs=q2n[:, :],
                                                        start=True, stop=True), reads=[k2TB, q2nB], writes=[PSB[4]])
                k.op("scalar", lambda e, mt=mt: e.activation(out=E2[mt][:, :], in_=PS[4][:, :], func=AF.Exp, scale=128.0 ** -0.5),
                     reads=[PSB[4]], writes=[E2B[mt]])
            for tt in range(4):
                for mt in range(2):
                    k.op("tensor", lambda e, tt=tt, mt=mt: e.matmul(PS[5][:, tt * 128:(tt + 1) * 128],
                                                                   lhsT=E2[mt][:, tt * 128:(tt + 1) * 128], rhs=V2[:, mt, h, :],
                                                                   start=(mt == 0), stop=(mt == 1), skip_group_check=True),
                         reads=[E2B[mt], V2B], writes=[PSB[5]])
                for mt in range(2):
                    k.op("tensor", lambda e, tt=tt, mt=mt: e.matmul(PS[6][:, tt:tt + 1], lhsT=E2[mt][:, tt * 128:(tt + 1) * 128],
                                                                   rhs=ones[:, 0:1], start=(mt == 0), stop=(mt == 1),
                                                                   skip_group_check=True),
                         reads=[E2B[mt], onesB], writes=[PSB[6]])
            k.op("vector", lambda e: e.reciprocal(out=rinv2[:, :], in_=PS[6][:, 0:4]), reads=[PSB[6]], writes=[rinv2B])
            for tt in range(4):
                k.op("vector", lambda e, tt=tt: e.tensor_scalar(out=o2[:, tt, h * 128:(h + 1) * 128], in0=PS[5][:, tt * 128:(tt + 1) * 128],
                                                               scalar1=rinv2[:, tt:tt + 1], scalar2=None, op0=ALU.mult),
                     reads=[PSB[5], rinv2B], writes=[o2B])

        c_proj(0)
        for h in range(4):
            if h + 1 < 4:
                c_proj(h + 1)
            c_rest(h)
        for tt in range(4):
            sl = 4 * G + tt
            for c in range(4):
                k.op("tensorT", lambda e, c=c, tt=tt: e.transpose(out=psb[:, c * 128:(c + 1) * 128], in_=o2[:, tt, c * 128:(c + 1) * 128],
                                                                identity=ident[:]), reads=[o2B, identB], writes=[psbB])
            k.op("scalar", lambda e: e.activation(out=o2T[:, :, :], in_=psb[:, 0:512].rearrange("p (a b) -> p a b", b=128), func=AF.Copy),
                 reads=[psbB], writes=[o2TB])
            for half in range(2):
                hb = half
                for c in range(4):
                    k.op("tensor", lambda e, c=c, half=half, hb=hb: e.matmul(PS[hb][:, :], lhsT=o2T[:, c, :],
                                                                             rhs=Wo2[:, c, half * 512:(half + 1) * 512],
                                                                             start=(c == 0), stop=(c == 3)),
                         reads=[o2TB, Wo2B], writes=[PSB[hb]])
                k.op("vector", lambda e, sl=sl, half=half, hb=hb: e.tensor_tensor(
                    out=xres[:, sl, half * 512:(half + 1) * 512], in0=xres[:, sl, half * 512:(half + 1) * 512], in1=PS[hb][:, :], op=ALU.add),
                    reads=[PSB[hb], xresB[sl]], writes=[xresB[sl]])

    if stop_after == "C":
        for sl in range(NOWN):
            k.dma("sync", lambda e, sl=sl: e.dma_start(out=out_d[sl * 128:(sl + 1) * 128, :], in_=xres[:, sl, :]), reads=[xresB[sl]])
        k.barrier()
        k.final_wait("sync")
        k.emit()
        m.release(0)
        k.close()
        return nc

    k.barrier()
    k.emit()
    m.release(mk_C)
    xn = [m.sb("xnD%d" % i, [128, D], BF16) for i in range(2)]; xnB = [Buf("xnD%d" % i) for i in range(2)]
    sqj = m.sb("sqjD", [128, D], BF16); sqjB = Buf("sqjD")
    stat = [m.sb("statD%d" % i, [128, 4], F32) for i in range(2)]; statB = [Buf("statD%d" % i) for i in range(2)]
    stg_set["bufs"] = stg; stg_set["B"] = stgB; stg_set["N"] = STG_N
    g3bc = m.sb("g3bc", [128, D], F32); g3B = Buf("g3bc")
    Wr = m.sb("Wr", [128, 8, 36], BF16); WrB = Buf("Wr")
    rbias = m.sb("rbias", [128, 36], F32); rbB = Buf("rbias")
    h3T = m.sb("h3T", [128, 8, NOWN * 128], BF16); h3TB = [Buf("h3T%d" % i) for i in range(NOWN)]
    gates = m.sb("gates", [128, NOWN, 32], F32); gatesB = [Buf("gates%d" % i) for i in range(NOWN)]
    lg = m.sb("lg", [128, 36], F32); lgB = Buf("lg")
    rt = m.sb("rt", [128, 64], F32); rtB = Buf("rt")
    k.dma("sync", lambda e: e.dma_start(out=g3bc[:], in_=g3bc_d), writes=[g3B])
    k.dma("sync", lambda e: e.dma_start(out=rbias[:], in_=rbias_d), writes=[rbB])
    for kc in range(8):
        load_cast(lambda lo, hi, kc=kc: Wr[:, kc, lo:hi], lambda lo, hi, kc=kc: wr_d[kc * 128:(kc + 1) * 128, lo:hi], 36, WrB)

    lgall = m.sb("lgall", [128, NOWN, 36], F32); lgallB = [Buf("lgall%d" % i) for i in range(NOWN)]
    for sl in range(NOWN):
        xb = sl % 2
        norm_transpose(xres[:, sl, :], g3bc, g3B, None, xresB[sl], xn[xb], xnB[xb], sqj, sqjB, stat[xb], statB[xb],
                       h3T[:, :, sl * 128:(sl + 1) * 128], h3TB[sl])
        for kc in range(8):
            k.op("tensor", lambda e, kc=kc, sl=sl: e.matmul(PS[6][:, 0:36], lhsT=h3T[:, kc, sl * 128:(sl + 1) * 128], rhs=Wr[:, kc, :],
                                                           start=(kc == 0), stop=(kc == 7)), reads=[h3TB[sl], WrB], writes=[PSB[6]])
        k.op("vector", lambda e, sl=sl: e.tensor_tensor(out=lgall[:, sl, :], in0=PS[6][:, 0:36], in1=rbias[:, :], op=ALU.add),
             reads=[PSB[6], rbB], writes=[lgallB[sl]])

    def route(sl):
        lgv = lgall[:, sl, :]

        def V_(eng, fn, reads=(), writes=()):
            k.op(eng, fn, reads=list(reads) + [rtB, lgallB[sl]], writes=list(writes) + [rtB])

        V_("vector", lambda e: e.tensor_reduce(out=rt[:, 0:1], in_=lgv[:, 0:4], axis=AX.X, op=ALU.max))
        V_("vector", lambda e: e.tensor_scalar(out=rt[:, 1:2], in0=rt[:, 0:1], scalar1=-1.0, scalar2=None, op0=ALU.mult))
        V_("vector", lambda e: e.tensor_scalar(out=rt[:, 4:8], in0=lgv[:, 0:4], scalar1=rt[:, 0:1], scalar2=None, op0=ALU.is_ge))
        V_("scalar", lambda e: e.activation(out=rt[:, 8:12], in_=lgv[:, 0:4], func=AF.Exp, bias=rt[:, 1:2], accum_out=rt[:, 2:3]))
        V_("vector", lambda e: e.reciprocal(out=rt[:, 3:4], in_=rt[:, 2:3]))
        V_("vector", lambda e: e.tensor_scalar(out=rt[:, 12:20], in0=lgv[:, 4:12], scalar1=rt[:, 4:5], scalar2=None, op0=ALU.mult))
        for g in range(1, 4):
            V_("vector", lambda e, g=g: e.scalar_tensor_tensor(out=rt[:, 12:20], in0=lgv[:, 4 + 8 * g:12 + 8 * g], scalar=rt[:, 4 + g:5 + g],
                                                              in1=rt[:, 12:20], op0=ALU.mult, op1=ALU.add))
        V_("vector", lambda e: e.tensor_reduce(out=rt[:, 20:21], in_=rt[:, 12:20], axis=AX.X, op=ALU.max))
        V_("vector", lambda e: e.tensor_scalar(out=rt[:, 21:29], in0=rt[:, 12:20], scalar1=rt[:, 20:21], scalar2=None, op0=ALU.is_ge))
        V_("vector", lambda e: e.scalar_tensor_tensor(out=rt[:, 29:37], in0=rt[:, 21:29], scalar=-1e30, in1=rt[:, 12:20],
                                                      op0=ALU.mult, op1=ALU.add))
        V_("vector", lambda e: e.tensor_reduce(out=rt[:, 37:38], in_=rt[:, 29:37], axis=AX.X, op=ALU.max))
        V_("vector", lambda e: e.tensor_scalar(out=rt[:, 38:46], in0=rt[:, 29:37], scalar1=rt[:, 37:38], scalar2=None, op0=ALU.is_ge))
        V_("vector", lambda e: e.tensor_tensor(out=rt[:, 46:47], in0=rt[:, 37:38], in1=rt[:, 20:21], op=ALU.subtract))
        V_("scalar", lambda e: e.activation(out=rt[:, 47:48], in_=rt[:, 46:47], func=AF.Exp))
        V_("vector", lambda e: e.tensor_scalar(out=rt[:, 48:49], in0=rt[:, 47:48], scalar1=1.0, scalar2=None, op0=ALU.add))
        V_("vector", lambda e: e.reciprocal(out=rt[:, 49:50], in_=rt[:, 48:49]))
        V_("vector", lambda e: e.tensor_tensor(out=rt[:, 50:51], in0=rt[:, 47:48], in1=rt[:, 49:50], op=ALU.mult))
        V_("vector", lambda e: e.tensor_tensor(out=rt[:, 51:52], in0=rt[:, 49:50], in1=rt[:, 3:4], op=ALU.mult))
        V_("vector", lambda e: e.tensor_tensor(out=rt[:, 52:53], in0=rt[:, 50:51], in1=rt[:, 3:4], op=ALU.mult))
        V_("vector", lambda e: e.tensor_scalar(out=rt[:, 53:61], in0=rt[:, 21:29], scalar1=rt[:, 51:52], scalar2=None, op0=ALU.mult))
        V_("vector", lambda e: e.scalar_tensor_tensor(out=rt[:, 53:61], in0=rt[:, 38:46], scalar=rt[:, 52:53], in1=rt[:, 53:61],
                                                      op0=ALU.mult, op1=ALU.add))
        for g in range(4):
            k.op("vector", lambda e, g=g, sl=sl: e.tensor_scalar(out=gates[:, sl, g * 8:(g + 1) * 8], in0=rt[:, 53:61],
                                                                scalar1=rt[:, 4 + g:5 + g], scalar2=None, op0=ALU.mult),
                 reads=[rtB], writes=[gatesB[sl]])

    Wgu = [m.sb("Wgu%d" % i, [128, 8, 512], BF16) for i in range(2)]; WguB = [Buf("Wgu%d" % i) for i in range(2)]
    Wd = [m.sb("Wd%d" % i, [128, 2, D], BF16) for i in range(2)]; WdB = [Buf("Wd%d" % i) for i in range(2)]
    est = [m.sb("est%d" % i, [128, 2048], F32) for i in range(3)]; estB = [Buf("est%d" % i) for i in range(3)]
    sa = [m.sb("sa%d" % i, [128, 512], BF16) for i in range(2)]; saB = [Buf("sa%d" % i) for i in range(2)]
    hidT = [m.sb("hidT%d" % i, [128, 2, 512], BF16) for i in range(2)]; hidTB = [Buf("hidT%d" % i) for i in range(2)]
    NE = int(os.environ.get("MK_NE", str(N_EXP)))
    est_i = [0]

    def load_expert(e_):
        eb = e_ % 2
        for which in range(3):
            i = est_i[0] % 3
            est_i[0] += 1
            if which < 2:
                src = (wg_d if which == 0 else wu_d)[e_].rearrange("(kc p) f -> p kc f", p=128)
                k.dma("sync", lambda e, i=i, src=src: e.dma_start(out=est[i][:, :].rearrange("p (kc f) -> p kc f", kc=8), in_=src),
                      writes=[estB[i]])
                k.op("gpsimd", lambda e, i=i, eb=eb, which=which: e.tensor_copy(
                    out=Wgu[eb][:, :, which * 256:(which + 1) * 256], in_=est[i][:, :].rearrange("p (kc f) -> p kc f", kc=8)),
                    reads=[estB[i]], writes=[WguB[eb]])
            else:
                src = wd_d[e_].rearrange("(fc p) d -> p fc d", p=128)
                k.dma("sync", lambda e, i=i, src=src: e.dma_start(out=est[i][:, :].rearrange("p (fc d) -> p fc d", fc=2), in_=src),
                      writes=[estB[i]])
                k.op("gpsimd", lambda e, i=i, eb=eb: e.tensor_copy(out=Wd[eb][:, :, :], in_=est[i][:, :].rearrange("p (fc d) -> p fc d", fc=2)),
                     reads=[estB[i]], writes=[WdB[eb]])

    units = [(e_, G) for e_ in range(NE) for G in range(4)]
    dcount = [0]

    def emit_gu_block(e_, G, ub, fi):
        eb = e_ % 2
        col = (fi // 2) * 256 + (fi % 2) * 128
        for kc in range(8):
            k.op("tensor", lambda e, kc=kc: e.matmul(
                PS[fi][:, :], lhsT=Wgu[eb][:, kc, col:col + 128], rhs=h3T[:, kc, G * 512:(G + 1) * 512],
                start=(kc == 0), stop=(kc == 7)), reads=[WguB[eb]] + h3TB[4 * G:4 * G + 4], writes=[PSB[fi]])
        if fi < 2:
            k.op("scalar", lambda e: e.activation(out=sa[fi][:, :], in_=PS[fi][:, :], func=AF.Silu),
                 reads=[PSB[fi]], writes=[saB[fi]])
        else:
            fc = fi - 2
            k.op("vector", lambda e: e.tensor_tensor(out=hidT[ub][:, fc, :], in0=PS[fi][:, :], in1=sa[fc][:, :], op=ALU.mult),
                 reads=[PSB[fi], saB[fc]], writes=[hidTB[ub]])

    def emit_down_pair(e_, G, ub, t4, half):
        eb = e_ % 2
        tt = 4 * G + t4
        db = 4 + dcount[0] % 3
        dcount[0] += 1
        for fc in range(2):
            k.op("tensor", lambda e, fc=fc: e.matmul(
                PS[db][:, :], lhsT=hidT[ub][:, fc, t4 * 128:(t4 + 1) * 128], rhs=Wd[eb][:, fc, half * 512:(half + 1) * 512],
                start=(fc == 0), stop=(fc == 1)), reads=[hidTB[ub], WdB[eb]], writes=[PSB[db]])
        k.op("vector", lambda e: e.scalar_tensor_tensor(
            out=xres[:, tt, half * 512:(half + 1) * 512], in0=PS[db][:, :], scalar=gates[:, tt, e_:e_ + 1],
            in1=xres[:, tt, half * 512:(half + 1) * 512], op0=ALU.mult, op1=ALU.add),
            reads=[PSB[db], xresB[tt], gatesB[tt]], writes=[xresB[tt]])

    load_expert(0)
    for ui in range(len(units) + 1):
        cur = units[ui] if ui < len(units) else None
        prev = units[ui - 1] if ui >= 1 else None
        if prev is not None and prev[0] == 0:
            for t4 in range(4):
                route(4 * prev[1] + t4)
        for fi in range(4):
            if cur is not None:
                emit_gu_block(cur[0], cur[1], ui % 2, fi)
            if prev is not None:
                for q_ in range(2):
                    pi = 2 * fi + q_
                    emit_down_pair(prev[0], prev[1], (ui - 1) % 2, pi // 2, pi % 2)
        if cur is not None and cur[1] == 0 and cur[0] + 1 < NE:
            load_expert(cur[0] + 1)

    for sl in range(NOWN):
        k.dma("sync", lambda e, sl=sl: e.dma_start(out=out_d[sl * 128:(sl + 1) * 128, :], in_=xres[:, sl, :]), reads=[xresB[sl]])
    k.barrier()
    k.final_wait("sync")
    k.emit()
    m.release(0)
    k.close()
    return nc


def make_in_maps(inputs):
    x = np.asarray(inputs["x"], np.float32)
    mem = np.asarray(inputs["mem"], np.float32)
    w_in = np.asarray(inputs["w_in"], np.float32)[0]
    cosA, sinA = rope_tables()
    w_inA = np.ascontiguousarray(np.concatenate(
        [w_in[:, 0:512], w_in[:, 1536:2048], w_in[:, 1024:1536], w_in[:, 2560:2624], w_in[:, 2560:2624]], axis=1))
    w_inB = np.ascontiguousarray(np.concatenate([w_in[:, 512:1024], w_in[:, 2048:2560], w_in[:, 2624:2632]], axis=1))

    def bc(v):
        return np.ascontiguousarray(np.broadcast_to(np.asarray(v, np.float32).reshape(1, -1), (128, v.size)))

    gvec = np.zeros((128, 8), np.float32)
    gvec[:, 0] = np.tile(np.asarray(inputs["q_norm"], np.float32)[0], 2)
    gvec[:, 1] = np.tile(np.asarray(inputs["k_norm"], np.float32)[0], 2)
    gvec[:, 2] = np.tile(np.asarray(inputs["idx_k_norm"], np.float32)[0], 2)
    gvec[:, 3] = np.asarray(inputs["xattn_q_norm"], np.float32)[0]
    gvec[:, 4] = np.asarray(inputs["xattn_k_norm"], np.float32)[0]
    blk1 = np.zeros((128, 128), np.float32)
    blk1[:64, :64] = 1.0
    blk1[64:, 64:] = 1.0
    poolw = np.ascontiguousarray(np.asarray(inputs["pool_w"], np.float32)[0].transpose(1, 0, 2))
    pscale = np.ascontiguousarray(np.asarray(inputs["pool_scale"], np.float32)[0].reshape(4, 128).T)
    wr = np.ascontiguousarray(np.concatenate([np.asarray(inputs["router_group_w"], np.float32)[0],
                                              np.asarray(inputs["router_expert_w"], np.float32)[0]], axis=1))
    rb = np.concatenate([np.asarray(inputs["router_group_b"], np.float32)[0],
                         np.asarray(inputs["router_expert_b"], np.float32)[0]])
    shared = {
        "w_inA": w_inA, "w_inB": w_inB,
        "g1bc": bc(np.asarray(inputs["mix_norm"])[0]), "g2bc": bc(np.asarray(inputs["xattn_norm"])[0]),
        "g3bc": bc(np.asarray(inputs["ffn_norm"])[0]), "gmbc": bc(np.asarray(inputs["mem_norm"])[0]),
        "g1col": np.ascontiguousarray(np.asarray(inputs["mix_norm"], np.float32)[0].reshape(8, 128).T),
        "gvec": gvec, "ident": np.eye(128, dtype=np.float32), "blk1": blk1, "ones": np.ones((128, 128), np.float32),
        "rot": rot_matrix(), "cosA": cosA, "sinA": sinA, "poolw": poolw, "pscale": pscale,
        "w_out": np.asarray(inputs["w_out"], np.float32)[0], "xwq": np.asarray(inputs["xattn_wq"], np.float32)[0],
        "xwkv": np.asarray(inputs["xattn_wkv"], np.float32)[0], "xwo": np.asarray(inputs["xattn_wo"], np.float32)[0],
        "wrouter": wr, "rbias": bc(rb),
        "e_wg": np.asarray(inputs["expert_w_gate"], np.float32)[0].reshape(N_EXP, D, 256),
        "e_wu": np.asarray(inputs["expert_w_up"], np.float32)[0].reshape(N_EXP, D, 256),
        "e_wd": np.asarray(inputs["expert_w_down"], np.float32)[0].reshape(N_EXP, 256, D),
    }
    per_half = []
    for half in range(2):
        ot = own_tiles(half)
        tok = np.concatenate([np.arange(t * 128, (t + 1) * 128) for t in ot])
        bt = band_tables(half).reshape(48, 128, 128).transpose(1, 0, 2)
        ad = adm_tables(half).reshape(4, 128, 128).transpose(1, 0, 2)
        per_half.append({"tok": tok, "cosO": np.ascontiguousarray(cosA[:, tok]), "sinO": np.ascontiguousarray(sinA[:, tok]),
                         "band": np.ascontiguousarray(bt), "adm": np.ascontiguousarray(ad)})
    in_maps = []
    for c in range(8):
        b, half = c // 2, c % 2
        ph = per_half[half]
        mp = dict(shared)
        mp["x_all"] = np.ascontiguousarray(x[b])
        mp["x_own"] = np.ascontiguousarray(x[b][ph["tok"]])
        mp["mem_b"] = np.ascontiguousarray(mem[b])
        mp["cosO"] = ph["cosO"]; mp["sinO"] = ph["sinO"]; mp["band"] = ph["band"]; mp["adm"] = ph["adm"]
        in_maps.append(mp)
    return in_maps, per_half


_NC_CACHE = {}


def kernel(**inputs):
    in_maps, per_half = make_in_maps(inputs)
    if "nc" not in _NC_CACHE:
        _NC_CACHE["nc"] = build_program()
    nc = _NC_CACHE["nc"]
    in_maps = [{n: mp[n] for n in nc._mk_in_names} for mp in in_maps]
    res = run_bass_kernel_spmd(nc, in_maps, core_ids=list(range(8)))
    out = np.zeros((4, S, D), np.float32)
    for c in range(8):
        b, half = c // 2, c % 2
        out[b][per_half[half]["tok"]] = np.asarray(res.results[c]["out"], np.float32)
    return out
```
